# Optimizing a Trainium2 kernel written in Bass

```python
import jax
import jax.numpy as jnp
from jax import lax
import numpy as np

D_MODEL = 1024
BATCH = 16
SEQ = 2048
DEPTH = 1

MIX_WIDTH = D_MODEL
GMLP_CHUNK = 128
GMLP_GROUPS = 8
GMLP_GROUP_DIM = MIX_WIDTH // GMLP_GROUPS
HGRN_HEADS = 8
HGRN_HEAD_DIM = MIX_WIDTH // HGRN_HEADS
HGRN_CHUNK = 64
N_IN_SLICES = 8
N_GROUPS = 4
EXPERTS_PER_GROUP = 4
N_EXPERTS = N_GROUPS * EXPERTS_PER_GROUP
EXPERT_FF = D_MODEL // 2
TOP_K_IN_GROUP = 2
RMS_EPS = 1e-6
LN_EPS = 1e-5

kernel_name = "hybrid_gmlp_hgrn2_hmoe_block"


def _rmsnorm(x, g):
    xf = x.astype(jnp.float32)
    y = xf * lax.rsqrt(jnp.mean(xf * xf, axis=-1, keepdims=True) + RMS_EPS)
    return (y * g.astype(jnp.float32)).astype(x.dtype)


def _layernorm(x, g, b):
    xf = x.astype(jnp.float32)
    mu = jnp.mean(xf, axis=-1, keepdims=True)
    var = jnp.mean(jnp.square(xf - mu), axis=-1, keepdims=True)
    y = (xf - mu) * lax.rsqrt(var + LN_EPS)
    return (y * g.astype(jnp.float32) + b.astype(jnp.float32)).astype(x.dtype)


def _hgrn2_chunk_scan(q, k, v, log_f):
    b_, s_, h_, kd = q.shape
    vd = v.shape[-1]
    n = s_ // HGRN_CHUNK

    def to_chunks(t):
        return t.astype(jnp.float32).reshape(b_, n, HGRN_CHUNK, h_, t.shape[-1]).transpose(1, 0, 3, 2, 4)

    qc, kc, vc, fc = to_chunks(q), to_chunks(k), to_chunks(v), to_chunks(log_f)
    causal = jnp.tril(jnp.ones((HGRN_CHUNK, HGRN_CHUNK), dtype=bool))[:, :, None]

    def step(state, inp):
        q_, k_, v_, lf = inp
        bcum = jnp.cumsum(lf, axis=2)
        diff = bcum[:, :, :, None, :] - bcum[:, :, None, :, :]
        decay = jnp.exp(jnp.where(causal, diff, -jnp.inf))
        scores = jnp.einsum('bhtsk,bhsk->bhts', q_[:, :, :, None, :] * decay, k_)
        o = (jnp.einsum('bhts,bhsv->bhtv', scores, v_)
             + jnp.einsum('bhtk,bhkv->bhtv', q_ * jnp.exp(bcum), state))
        b_last = bcum[:, :, -1:, :]
        state = (jnp.exp(b_last[:, :, 0, :])[..., None] * state
                 + jnp.einsum('bhsk,bhsv->bhkv', k_ * jnp.exp(b_last - bcum), v_))
        return state, o

    s0 = jnp.zeros((b_, h_, kd, vd), jnp.float32)
    _, o = lax.scan(step, s0, (qc, kc, vc, fc))
    return o.transpose(1, 0, 3, 2, 4).reshape(b_, s_, h_, vd)


def _token_mixer(h, w_in, ln_v_g, ln_v_b, w_spatial, b_spatial, lb, g_hgrn, w_out):
    b_, s_, _ = h.shape
    proj = h @ w_in
    u, v, q, f_raw, i, og, gate_a, gate_b = jnp.split(proj, N_IN_SLICES, axis=-1)

    u = jax.nn.gelu(u)
    v = _layernorm(jax.nn.gelu(v), ln_v_g, ln_v_b)
    n = s_ // GMLP_CHUNK
    vc = v.reshape(b_, n, GMLP_CHUNK, GMLP_GROUPS, GMLP_GROUP_DIM)
    w_causal = w_spatial * jnp.tril(jnp.ones((GMLP_CHUNK, GMLP_CHUNK), w_spatial.dtype))
    zv = jnp.einsum('gts,bnsgc->bntgc', w_causal, vc) + b_spatial.T[:, :, None]
    y_a = u * zv.reshape(b_, s_, MIX_WIDTH)

    hs = (b_, s_, HGRN_HEADS, HGRN_HEAD_DIM)
    qh = jax.nn.silu(q).reshape(hs)
    f = lb + (1.0 - lb) * jax.nn.sigmoid(f_raw.astype(jnp.float32))
    log_f = jnp.log(f).reshape(hs)
    kh = (1.0 - f).reshape(hs)
    o = _hgrn2_chunk_scan(qh, kh, i.reshape(hs), log_f)
    o = _rmsnorm(o, g_hgrn.reshape(HGRN_HEADS, HGRN_HEAD_DIM))
    y_b = o.reshape(b_, s_, MIX_WIDTH).astype(h.dtype) * jax.nn.silu(og)

    y = jax.nn.sigmoid(gate_a) * y_a + jax.nn.sigmoid(gate_b) * y_b
    return y @ w_out


def _hier_moe(h, w_rg, b_rg, w_re, b_re, w1, w3, w2):
    b_, s_, _ = h.shape
    p_group = jax.nn.softmax((h @ w_rg + b_rg).astype(jnp.float32), axis=-1)
    p_top, g_idx = lax.top_k(p_group, 1)
    e_logits = (h @ w_re + b_re).astype(jnp.float32).reshape(b_, s_, N_GROUPS, EXPERTS_PER_GROUP)
    sel = jax.nn.one_hot(g_idx[..., 0], N_GROUPS, dtype=jnp.float32)
    e_logits = jnp.einsum('bsge,bsg->bse', e_logits, sel)
    p_exp = jax.nn.softmax(e_logits, axis=-1)
    top_p, top_i = lax.top_k(p_exp, TOP_K_IN_GROUP)
    w = p_top * top_p / jnp.sum(top_p, axis=-1, keepdims=True)
    expert_id = g_idx * EXPERTS_PER_GROUP + top_i
    combine = jnp.sum(w[..., None] * jax.nn.one_hot(expert_id, N_EXPERTS, dtype=jnp.float32), axis=-2)
    combine = combine.astype(h.dtype)
    y = jnp.zeros_like(h)
    for e in range(N_EXPERTS):
        a = jax.nn.silu(h @ w1[e]) * (h @ w3[e])
        y = y + combine[..., e:e + 1] * (a @ w2[e])
    return y


def setup_inputs(seed: int = 0) -> dict:
    key = jax.random.key(seed)
    ks = jax.random.split(key, 23)

    def nrm(k, shape, scale):
        return jax.random.normal(k, shape, jnp.float32) * scale

    d, wd, c_ = D_MODEL, MIX_WIDTH, GMLP_CHUNK
    return {
        "x": nrm(ks[0], (BATCH, SEQ, d), 1.0),
        "c": nrm(ks[1], (BATCH, d), 1.0),
        "w_ada": nrm(ks[2], (DEPTH, d, 6 * d), 0.5 * d ** -0.5),
        "b_ada": nrm(ks[3], (DEPTH, 6 * d), 0.01),
        "g_pre_mix": 1.0 + nrm(ks[4], (DEPTH, d), 0.05),
        "g_post_mix": 1.0 + nrm(ks[5], (DEPTH, d), 0.05),
        "w_in": nrm(ks[6], (DEPTH, d, N_IN_SLICES * wd), d ** -0.5),
        "ln_v_g": 1.0 + nrm(ks[7], (DEPTH, wd), 0.05),
        "ln_v_b": nrm(ks[8], (DEPTH, wd), 0.02),
        "w_spatial": nrm(ks[9], (DEPTH, GMLP_GROUPS, c_, c_), 0.5 * c_ ** -0.5),
        "b_spatial": 1.0 + nrm(ks[10], (DEPTH, GMLP_GROUPS, c_), 0.05),
        "lb_logits": nrm(ks[11], (DEPTH + 1, wd), 0.1),
        "g_hgrn_norm": 1.0 + nrm(ks[12], (DEPTH, wd), 0.05),
        "w_out": nrm(ks[13], (DEPTH, wd, d), wd ** -0.5),
        "g_pre_ffn": 1.0 + nrm(ks[14], (DEPTH, d), 0.05),
        "g_post_ffn": 1.0 + nrm(ks[15], (DEPTH, d), 0.05),
        "w_router_group": nrm(ks[16], (DEPTH, d, N_GROUPS), d ** -0.5),
        "b_router_group": nrm(ks[17], (DEPTH, N_GROUPS), 0.01),
        "w_router_expert": nrm(ks[18], (DEPTH, d, N_EXPERTS), d ** -0.5),
        "b_router_expert": nrm(ks[19], (DEPTH, N_EXPERTS), 0.01),
        "w1": nrm(ks[20], (DEPTH, N_EXPERTS, d, EXPERT_FF), d ** -0.5),
        "w3": nrm(ks[21], (DEPTH, N_EXPERTS, d, EXPERT_FF), d ** -0.5),
        "w2": nrm(ks[22], (DEPTH, N_EXPERTS, EXPERT_FF, d), EXPERT_FF ** -0.5),
    }


def reference(x, c, w_ada, b_ada, g_pre_mix, g_post_mix, w_in, ln_v_g, ln_v_b,
              w_spatial, b_spatial, lb_logits, g_hgrn_norm, w_out, g_pre_ffn,
              g_post_ffn, w_router_group, b_router_group, w_router_expert,
              b_router_expert, w1, w3, w2):
    lb_all = jnp.cumsum(jax.nn.softmax(lb_logits.astype(jnp.float32), axis=0), axis=0)
    for layer in range(DEPTH):
        ada = jax.nn.silu(c) @ w_ada[layer] + b_ada[layer]
        sh1, sc1, gt1, sh2, sc2, gt2 = jnp.split(ada[:, None, :], 6, axis=-1)

        h = _rmsnorm(x, g_pre_mix[layer]) * (1.0 + sc1) + sh1
        y = _token_mixer(h, w_in[layer], ln_v_g[layer], ln_v_b[layer], w_spatial[layer],
                         b_spatial[layer], lb_all[layer], g_hgrn_norm[layer], w_out[layer])
        x = x + gt1 * _rmsnorm(y, g_post_mix[layer])

        h = _rmsnorm(x, g_pre_ffn[layer]) * (1.0 + sc2) + sh2
        y = _hier_moe(h, w_router_group[layer], b_router_group[layer], w_router_expert[layer],
                      b_router_expert[layer], w1[layer], w3[layer], w2[layer])
        x = x + gt2 * _rmsnorm(y, g_post_ffn[layer])
    return x
```

```python
import contextlib
import numpy as np
import concourse.bass as bass
import concourse.mybir as mybir
from concourse.bass_utils import run_bass_kernel_spmd

F32 = mybir.dt.float32
BF16 = mybir.dt.bfloat16
AF = mybir.ActivationFunctionType
ALU = mybir.AluOpType
AX = mybir.AxisListType

D = 1024
NCORES = 8
NE = 16
FF = 512
ENGS = ("pe", "act", "dve", "pool", "sp")
STOP = None


class Buf:
    __slots__ = ("name", "writers", "readers", "sem_idx", "dcount")

    def __init__(self, name):
        self.name = name
        self.writers = {}
        self.readers = {}
        self.sem_idx = None
        self.dcount = 0


class Op:
    __slots__ = ("id", "eng", "fn", "deps", "kind", "val", "dsem", "dval")


class Plan:
    def __init__(self):
        self.ops = []
        self.n_dma_sems = 0
        self.out_ops = []

    def _mk(self, eng, fn, kind):
        op = Op()
        op.id = len(self.ops)
        op.eng = eng
        op.fn = fn
        op.kind = kind
        op.deps = []
        op.val = None
        op.dsem = None
        op.dval = None
        self.ops.append(op)
        return op

    @staticmethod
    def _key(op):
        return ("d", op.dsem) if op.kind == "d" else ("c", op.eng)

    def _track(self, op, r, w):
        deps = {}
        for b in r:
            for o in b.writers.values():
                deps[o.id] = o
        for b in w:
            for o in b.writers.values():
                deps[o.id] = o
            for o in b.readers.values():
                deps[o.id] = o
        op.deps = [o for o in deps.values() if not (o.kind == "c" and o.eng == "pe" and op.kind == "c" and op.eng == "pe")]
        k = self._key(op)
        for b in r:
            b.readers[k] = op
        for b in w:
            b.writers[k] = op
            b.readers = {}

    def c(self, eng, fn, r=(), w=(), mode=None):
        op = self._mk(eng, fn, "c")
        self._track(op, r, w)
        if eng == "pe":
            last = getattr(self, "_last_pe", None)
            if last is not None and last[1] != mode and all(d.id != last[0].id for d in op.deps):
                op.deps.append(last[0])
            self._last_pe = (op, mode)
        return op

    def dma(self, eng, fn, dst, r=(), w=(), is_out=False):
        op = self._mk(eng, fn, "d")
        if dst.sem_idx is None:
            dst.sem_idx = self.n_dma_sems
            self.n_dma_sems += 1
        dst.dcount += 16
        op.dsem = dst.sem_idx
        op.dval = dst.dcount
        self._track(op, r, w)
        if is_out:
            self.out_ops.append(op)
        return op

    def lower(self, nc):
        ops = self.ops
        needed = set()
        for op in ops:
            for d in op.deps:
                if d.kind == "c":
                    needed.add(d.id)
        cnt = {e: 0 for e in ENGS}
        for op in ops:
            if op.kind == "c" and op.id in needed:
                cnt[op.eng] += 1
                op.val = cnt[op.eng]
        per_eng = {e: [o for o in ops if o.eng == e] for e in ENGS}
        out_ops = self.out_ops
        with contextlib.ExitStack() as st:
            esem = {e: st.enter_context(nc.semaphore("s_" + e)) for e in ENGS}
            dsem = [st.enter_context(nc.semaphore("d%d" % i)) for i in range(self.n_dma_sems)]
            block = st.enter_context(nc.Block())

            def run(h, e):
                waited = {}

                def wait_for(d):
                    if d.kind == "c":
                        key, sem, val = ("c", d.eng), esem[d.eng], d.val
                    else:
                        key, sem, val = ("d", d.dsem), dsem[d.dsem], d.dval
                    if waited.get(key, 0) < val:
                        h.wait_ge(sem, val)
                        waited[key] = val

                for op in per_eng[e]:
                    for d in op.deps:
                        wait_for(d)
                    inst = op.fn(h)
                    if op.kind == "d":
                        inst.then_inc(dsem[op.dsem], 16)
                    elif op.id in needed:
                        inst.then_inc(esem[e], 1)
                if e == "sp":
                    for d in out_ops:
                        wait_for(d)

            @block.tensor
            def _(h):
                run(h, "pe")

            @block.scalar
            def _(h):
                run(h, "act")

            @block.vector
            def _(h):
                run(h, "dve")

            @block.gpsimd
            def _(h):
                run(h, "pool")

            @block.sync
            def _(h):
                run(h, "sp")


class Reg:
    __slots__ = ("ap", "bufs")

    def __init__(self, ap, bufs):
        self.ap = ap
        self.bufs = list(bufs)


def build(S=2048, upto=9, debug=False):
    NT = S // 128
    NB = S // 512
    nc = bass.Bass("TRN2", target_bir_lowering=False)
    P = Plan()

    def din(name, shape, dt=F32):
        return nc.dram_tensor(name, list(shape), dt, kind="ExternalInput").ap()

    x_d = din("x", [2, S, D])
    cT_d = din("cT", [128, 8, 2])
    wada_d = din("w_ada", [D, 6 * D])
    bada_d = din("b_ada2", [2, 6 * D])
    vecfm_d = din("vecfm", [128, 8, 8])
    gpm_d = din("g_post_mix_bc", [128, D])
    gpf_d = din("g_post_ffn_bc", [128, D])
    wfm_d = din("w_fm", [8, D, 896])
    wv_d = din("w_v", [D, D])
    wspT_d = din("w_spT", [128, 8, 128])
    lnb2_d = din("lnb2", [2, D])
    bsp_d = din("b_sp", [1, 8 * 128])
    wout_d = din("w_out", [D, D])
    wr_d = din("w_r", [D, 20])
    brbc_d = din("b_r_bc", [128, 20])
    w1_d = din("w1", [NE, D, FF])
    w3_d = din("w3", [NE, D, FF])
    w2_d = din("w2", [NE, FF, D])
    cst_d = din("cst", [128, 1024])
    out_d = nc.dram_tensor("out", [2, S, D], F32, kind="ExternalOutput").ap()
    x1s_d = nc.dram_tensor("x1s", [2, S, D], F32, kind="Internal").ap()
    dbg = {}
    if debug:
        dbg["hT"] = nc.dram_tensor("dbg_hT", [128, 8 * S], BF16, kind="ExternalOutput").ap()
        dbg["yT"] = nc.dram_tensor("dbg_yT", [128, 8 * S], BF16, kind="ExternalOutput").ap()
        dbg["misc"] = nc.dram_tensor("dbg_misc", [128, 4096], F32, kind="ExternalOutput").ap()

    with contextlib.ExitStack() as st:
        def sb(name, cols, dt):
            return st.enter_context(nc.sbuf_tensor("sb_" + name, [128, cols], dt))

        cst = sb("cst", 768, F32)
        cstb = sb("cstb", 256, BF16)
        vecfm = sb("vecfm", 64, F32)
        brbc = sb("brbc", 20, F32)
        small = sb("small", 256, F32)
        adaT = sb("adaT", 96, F32)
        scl = sb("scl", 96, F32)
        gtbc = sb("gtbc", 2 * D, F32)
        wcT = sb("wcT", 1024, BF16)
        Rsb = sb("Rsb", 1024, F32)
        wrb = sb("wrb", 160, BF16)
        lg = sb("lg", NT * 20, F32)
        comb = sb("comb", NT * 16, F32)
        rtmp = sb("rtmp", NT * 64, F32)
        hT = sb("hT", 8 * S, BF16)
        XA = sb("XA", 16 * S, BF16)
        WA = sb("WA", 20480, BF16)
        TA = sb("TA", 10240, F32)
        QFt = sb("QF", 512, F32)
        OFt = sb("OF", 512, F32)
        psum = [st.enter_context(nc.psum_tensor("ps%d" % i, [128, 1024], F32)) for i in range(4)]

        B = {}

        def buf(name):
            if name not in B:
                B[name] = Buf(name)
            return B[name]

        bank_bufs = [buf("bank%d" % i) for i in range(8)]
        bank_rr = [0]

        def bank():
            k = bank_rr[0] % 8
            bank_rr[0] += 1
            return Reg(psum[k // 2][:, (k % 2) * 512:(k % 2) * 512 + 512], [bank_bufs[k]])

        def bank2():
            if bank_rr[0] % 2:
                bank_rr[0] += 1
            k = bank_rr[0] % 8
            bank_rr[0] += 2
            return Reg(psum[k // 2][:, :], [bank_bufs[k], bank_bufs[k + 1]])

        TAb = [buf("ta%d" % i) for i in range(40)]

        def ta(off_f32, ncols, dt=F32):
            nf = ncols if dt == F32 else (ncols + 1) // 2
            ap = TA[:, off_f32:off_f32 + nf]
            if dt != F32:
                ap = ap.bitcast(dt)
            b0, b1 = off_f32 // 256, (off_f32 + nf - 1) // 256
            return Reg(ap, TAb[b0:b1 + 1])

        WAb = [buf("wa%d" % i) for i in range(5)]

        def wa(off, ncols):
            b0, b1 = off // 4096, (off + ncols - 1) // 4096
            return Reg(WA[:, off:off + ncols], WAb[b0:b1 + 1])

        XAb = [buf("xa%d" % i) for i in range(16 * S // 1024)]

        def xa(off, ncols):
            b0, b1 = off // 1024, (off + ncols - 1) // 1024
            return Reg(XA[:, off:off + ncols], XAb[b0:b1 + 1])

        XAf = XA[:, :].bitcast(F32)

        def xacc(j):
            return Reg(XAf[:, j * 1024:(j + 1) * 1024], XAb[2 * j:2 * j + 2])

        hTb = [buf("hT%d" % j) for j in range(NT)]
        hT3 = hT[:, :].rearrange("p (k s) -> p k s", s=S)

        ident_f = cst[:, 0:128]
        mask_bd = cst[:, 256:384]
        causal = cst[:, 384:512]
        ident_b = cstb[:, 0:128]
        ones_b = cstb[:, 128:256]
        bsmall = buf("small")
        badaT = buf("adaT")
        bscl = buf("scl")
        bvec = buf("vecfm")
        bgt = [buf("gt1bc"), buf("gt2bc")]
        bconst = buf("consts2")
        blg = buf("lg")
        bcomb = buf("comb")
        brt = buf("rtmp")

        def r32(n):
            return 32 if n <= 32 else (64 if n <= 64 else 128)

        def pmode(lhsT, kind="m"):
            return (kind, r32(lhsT.shape[0]), r32(lhsT.shape[-1]), str(lhsT.dtype))

        def mm(out, lhsT, rhs, start, stop, r, w):
            P.c("pe", lambda h: h.matmul(out, lhsT, rhs, start=start, stop=stop), r=r, w=w, mode=pmode(lhsT))

        def act(out, in_, func, r, w, **kw):
            P.c("act", lambda h: h.activation(out, in_, func, **kw), r=r, w=w)

        def dve(fn, r, w):
            P.c("dve", fn, r=r, w=w)

        def pool(fn, r, w):
            P.c("pool", fn, r=r, w=w)

        def ld(eng, out, in_, dst, w):
            P.dma(eng, lambda h: h.dma_start(out=out, in_=in_), dst, w=w)

        def tt(out, in0, in1, op, r, w):
            dve(lambda h: h.tensor_tensor(out=out, in0=in0, in1=in1, op=op), r, w)

        def ts(out, in0, s1, s2, op0, op1, r, w):
            if s2 is None:
                dve(lambda h: h.tensor_scalar(out=out, in0=in0, scalar1=s1, scalar2=None, op0=op0), r, w)
            else:
                dve(lambda h: h.tensor_scalar(out=out, in0=in0, scalar1=s1, scalar2=s2, op0=op0, op1=op1), r, w)

        def stt(out, in0, scalar, in1, op0, op1, r, w):
            dve(lambda h: h.scalar_tensor_tensor(out=out, in0=in0, scalar=scalar, in1=in1, op0=op0, op1=op1), r, w)

        def bc_last(ap2, n):
            return ap2.unsqueeze(2).to_broadcast([128, ap2.shape[1], n])

        def bc_mid(ap2, a):
            return ap2.unsqueeze(1).to_broadcast([128, a, ap2.shape[1]])

        ld("sp", cst[:, :], cst_d[:, 0:768], buf("cst"), [B["cst"]])
        ld("pool", cstb[:, 0:256], cst_d[:, 0:256], buf("cstb"), [B["cstb"]])
        ld("sp", vecfm[:, :], vecfm_d.rearrange("p a b -> p (a b)"), bvec, [bvec])
        ld("sp", brbc[:, :], brbc_d, buf("brbc"), [B["brbc"]])
        ld("pool", wrb[:, :].rearrange("p (k n) -> p k n", n=20), wr_d.rearrange("(k p) n -> p k n", p=128), buf("wrb"), [B["wrb"]])
        VF = vecfm[:, :].rearrange("p (a b) -> p a b", b=8)
        V_GPRE, V_LNG, V_LB0, V_LB1, V_GH, V_GPF, V_GPM, V_GPFF = range(8)

        EPS6 = small[:, 0:1]
        EPS5 = small[:, 1:2]
        dve(lambda h: h.memset(small[:, 0:1], 1e-6), [], [bsmall])
        dve(lambda h: h.memset(small[:, 1:2], 1e-5), [], [bsmall])
        EPS6X4 = small[:, 4:5]
        dve(lambda h: h.memset(small[:, 4:5], 4e-6), [], [bsmall])
        NEG1 = small[:, 2:3]
        NEGH = small[:, 3:4]
        dve(lambda h: h.memset(small[:, 2:3], -1.0), [], [bsmall])
        dve(lambda h: h.memset(small[:, 3:4], -0.5), [], [bsmall])
        FCA = small[:, 8:16]
        FCB = small[:, 16:24]
        GQ = small[:, 24:32]
        tt(small[:, 32:40], VF[:, V_LB0, :], VF[:, V_LB1, :], ALU.subtract, [bvec], [bsmall])
        act(small[:, 32:40], small[:, 32:40], AF.Tanh, [bsmall], [bsmall], scale=0.5)
        ts(FCA, small[:, 32:40], -0.25, 0.25, ALU.mult, ALU.add, [bsmall], [bsmall])
        ts(FCB, small[:, 32:40], 0.25, 0.75, ALU.mult, ALU.add, [bsmall], [bsmall])
        ts(GQ, VF[:, V_GH, :], 0.25, None, ALU.mult, None, [bvec], [bsmall])

        wsp = ta(0, 1024)
        ld("sp", wsp.ap, wspT_d.rearrange("p a b -> p (a b)"), wsp.bufs[0], wsp.bufs)
        tt(wcT[:, :].rearrange("p (g t) -> p g t", t=128), wsp.ap.rearrange("p (g t) -> p g t", t=128), bc_mid(causal, 8),
           ALU.mult, wsp.bufs + [B["cst"]], [bconst])
        l2 = ta(1024, 1024)
        r2t = ta(2048, 1024)
        ld("sp", l2.ap[0:2, :], lnb2_d, l2.bufs[0], l2.bufs)
        ld("sp", r2t.ap[1:2, :], bsp_d, r2t.bufs[0], r2t.bufs)
        pb2 = bank2()
        for half in range(2):
            mm(pb2.ap[0:1, half * 512:(half + 1) * 512], ones_b[:, 0:1], wcT[:, half * 512:(half + 1) * 512], True, True,
               [B["cstb"], bconst], pb2.bufs)
        dve(lambda h, pb2=pb2: h.tensor_copy(out=r2t.ap[0:1, :], in_=pb2.ap[0:1, :]), pb2.bufs, r2t.bufs)
        pb2 = bank2()
        for g in range(8):
            mm(pb2.ap[:, g * 128:(g + 1) * 128], l2.ap[0:2, g * 128:(g + 1) * 128], r2t.ap[0:2, g * 128:(g + 1) * 128], True, True,
               l2.bufs + r2t.bufs, pb2.bufs)
        dve(lambda h, pb2=pb2: h.tensor_copy(out=Rsb[:, :], in_=pb2.ap), pb2.bufs, [bconst])

        cTt = ta(3072, 16)
        ld("sp", cTt.ap, cT_d.rearrange("p a b -> p (a b)"), buf("cT"), cTt.bufs + [B["cT"]])
        thc = ta(3328, 16)
        act(thc.ap, cTt.ap, AF.Tanh, cTt.bufs + [B["cT"]], thc.bufs, scale=0.5)
        sc_b = ta(3584, 16, BF16)
        stt(thc.ap, thc.ap, 1.0, cTt.ap, ALU.add, ALU.mult, thc.bufs + cTt.bufs, thc.bufs)
        ts(sc_b.ap, thc.ap, 0.5, None, ALU.mult, None, thc.bufs, sc_b.bufs)
        scb3 = sc_b.ap.rearrange("p (k b) -> p k b", b=2)
        adar = ta(4096, 6144)
        adasb = adar.ap
        bada = adar.bufs
        ld("sp", adasb[0:2, :], bada_d, bada[0], bada)
        wada_v = wada_d.rearrange("(k p) n -> p k n", p=128)
        for nb in range(12):
            slot = wa((nb % 2) * 4096, 4096)
            w3v = slot.ap.rearrange("p (k n) -> p k n", n=512)
            ld("pool", w3v, wada_v[:, :, nb * 512:(nb + 1) * 512], slot.bufs[0], slot.bufs)
            pb = bank()
            for kc in range(8):
                mm(pb.ap[0:2, :], scb3[:, kc, :], w3v[:, kc, :], kc == 0, kc == 7, sc_b.bufs + slot.bufs, pb.bufs)
            tt(adasb[0:2, nb * 512:(nb + 1) * 512], pb.ap[0:2, :], adasb[0:2, nb * 512:(nb + 1) * 512], ALU.add,
               pb.bufs + bada, bada)
        for sl in (1, 4):
            ts(adasb[0:2, sl * D:(sl + 1) * D], adasb[0:2, sl * D:(sl + 1) * D], 1.0, None, ALU.add, None, bada, bada)
        pb = bank()
        for sl in range(6):
            for kc in range(8):
                col = (sl * 8 + kc) * 2
                mm(pb.ap[:, col:col + 2], adasb[0:2, sl * D + kc * 128: sl * D + (kc + 1) * 128], ident_f[0:2, 0:2],
                   True, True, bada + [B["cst"]], pb.bufs)
        dve(lambda h, pb=pb: h.tensor_copy(out=adaT[:, 0:96], in_=pb.ap[:, 0:96]), pb.bufs, [badaT])
        aT4 = adaT[:, 0:96].rearrange("p (s k b) -> p s k b", s=6, k=8)
        scl4 = scl[:, :].rearrange("p (s k b) -> p s k b", s=6, k=8)
        for b in range(2):
            tt(scl4[:, 0, :, b], aT4[:, 1, :, b], VF[:, V_GPRE, :], ALU.mult, [badaT, bvec], [bscl])
            tt(scl4[:, 2, :, b], aT4[:, 4, :, b], VF[:, V_GPF, :], ALU.mult, [badaT, bvec], [bscl])
            tt(scl4[:, 4, :, b], aT4[:, 2, :, b], VF[:, V_GPM, :], ALU.mult, [badaT, bvec], [bscl])
            tt(scl4[:, 5, :, b], aT4[:, 5, :, b], VF[:, V_GPFF, :], ALU.mult, [badaT, bvec], [bscl])
            dve(lambda h, b=b: h.tensor_copy(out=scl4[:, 1, :, b], in_=aT4[:, 0, :, b]), [badaT], [bscl])
            dve(lambda h, b=b: h.tensor_copy(out=scl4[:, 3, :, b], in_=aT4[:, 3, :, b]), [badaT], [bscl])

        def make_gtbc(which, b):
            dg = ta(9216, 1024)
            for kc in range(8):
                ts(dg.ap[:, kc * 128:(kc + 1) * 128], ident_f, scl4[:, 4 + which, kc, b:b + 1], None, ALU.mult, None,
                   [B["cst"], bscl], dg.bufs)
            pbg = bank2()
            for kc in range(8):
                mm(pbg.ap[:, kc * 128:(kc + 1) * 128], cst[:, 128:256], dg.ap[:, kc * 128:(kc + 1) * 128], True, True,
                   dg.bufs + [B["cst"]], pbg.bufs)
            dve(lambda h: h.tensor_copy(out=gtbc[:, which * D:(which + 1) * D], in_=pbg.ap), pbg.bufs, [bgt[which]])

        def phase1(s):
                ssq = small[:, 64:64 + NT]
                rstd = small[:, 80:80 + NT]
                xt = [ta(0, 1024), ta(1024, 1024)]
                junk = ta(2048, 1024, BF16)
                for j in range(NT):
                    xs = xt[j % 2]
                    ld("sp", xs.ap, x_d[s, j * 128:(j + 1) * 128, :], xs.bufs[0], xs.bufs)
                    act(junk.ap, xs.ap, AF.Square, xs.bufs, junk.bufs + [bsmall], accum_out=ssq[:, j:j + 1])
                act(rstd, ssq, AF.Sqrt, [bsmall], [bsmall], bias=EPS6, scale=1.0 / D)
                dve(lambda h: h.reciprocal(out=rstd, in_=rstd), [bsmall], [bsmall])
                xn = [ta(2560, 1024, BF16), ta(3072, 1024, BF16)]
                for j in range(NT):
                    xs = xt[j % 2]
                    ld("sp", xs.ap, x_d[s, j * 128:(j + 1) * 128, :], xs.bufs[0], xs.bufs)
                    xb = xn[j % 2]
                    act(xb.ap, xs.ap, AF.Copy, xs.bufs + [bsmall], xb.bufs, scale=rstd[:, j:j + 1])
                    pb = bank()
                    pbb = pb.ap.bitcast(BF16)
                    for kc in range(8):
                        P.c("pe", lambda h, pbb=pbb, xb=xb, kc=kc: h.transpose(pbb[:, kc * 128:(kc + 1) * 128],
                                                                                xb.ap[:, kc * 128:(kc + 1) * 128], ident_b),
                            r=xb.bufs + [B["cstb"]], w=pb.bufs, mode=("t", 128, 128))
                    for kc in range(8):
                        ts(hT3[:, kc, j * 128:(j + 1) * 128], pbb[:, kc * 128:(kc + 1) * 128],
                           scl4[:, 0, kc, s:s + 1], scl4[:, 1, kc, s:s + 1], ALU.mult, ALU.add, pb.bufs + [bscl], [hTb[j]])

        def prefetch_mixer_weights():
            wvs_ = wa(0, 8192)
            ld("pool", wvs_.ap.rearrange("p (k n) -> p k n", n=1024), wv_d.rearrange("(k p) n -> p k n", p=128), wvs_.bufs[0], wvs_.bufs)
            w0 = wa(8192, 7168)
            ld("pool", w0.ap.rearrange("p (k n) -> p k n", n=896), wfm_d[0].rearrange("(k p) n -> p k n", p=128), w0.bufs[0], w0.bufs)

        for s in range(2):
            if s == 0:
                prefetch_mixer_weights()
                phase1(0)
            if upto <= 1:
                break

            wvs = wa(0, 8192)
            wv3 = wvs.ap.rearrange("p (k n) -> p k n", n=1024)
            s1 = small[:, 96:96 + NT]
            s2 = small[:, 112:112 + NT]
            rsv = small[:, 128:128 + NT]
            nbv = small[:, 144:144 + NT]
            junk2 = ta(0, 1024, BF16)
            vn = [xa(j * 1024, 1024) for j in range(NT)]
            for j in range(NT):
                pb2 = bank2()
                for half in range(2):
                    for kc in range(8):
                        mm(pb2.ap[:, half * 512:(half + 1) * 512], hT3[:, kc, j * 128:(j + 1) * 128],
                           wv3[:, kc, half * 512:(half + 1) * 512], kc == 0, kc == 7, [hTb[j]] + wvs.bufs, pb2.bufs)
                act(vn[j].ap, pb2.ap, AF.Gelu_apprx_tanh, pb2.bufs, vn[j].bufs + [bsmall], accum_out=s1[:, j:j + 1])
                act(junk2.ap, vn[j].ap, AF.Square, vn[j].bufs, junk2.bufs + [bsmall], accum_out=s2[:, j:j + 1])
            ts(s1, s1, 1.0 / D, None, ALU.mult, None, [bsmall], [bsmall])
            ts(s2, s2, 1.0 / D, None, ALU.mult, None, [bsmall], [bsmall])
            tt(rsv, s1, s1, ALU.mult, [bsmall], [bsmall])
            tt(s2, s2, rsv, ALU.subtract, [bsmall], [bsmall])
            act(rsv, s2, AF.Sqrt, [bsmall], [bsmall], bias=EPS5, scale=1.0)
            dve(lambda h: h.reciprocal(out=rsv, in_=rsv), [bsmall], [bsmall])
            stt(nbv, s1, -1.0, rsv, ALU.mult, ALU.mult, [bsmall], [bsmall])
            for j in range(NT):
                act(vn[j].ap, vn[j].ap, AF.Identity, vn[j].bufs + [bsmall], vn[j].bufs, bias=nbv[:, j:j + 1], scale=rsv[:, j:j + 1])

            if STOP == "a":
                break
            YT0 = NT * 1024
            F_ = ta(0, 512); Pc = ta(512, 512); RP = ta(1024, 512); D1 = ta(1536, 512); THQ = ta(2048, 512)
            IT = ta(2560, 512, BF16); KH = ta(2816, 512, BF16)
            KP = [ta(3072 + 256 * i, 512, BF16) for i in range(2)]
            QH = [ta(3584 + 256 * i, 512, BF16) for i in range(2)]
            ITOK = [ta(4096 + 256 * i, 512, BF16) for i in range(2)]
            KHM = [[ta(4608, 512, BF16), ta(4864, 512, BF16)], [ta(8320, 512, BF16), ta(8576, 512, BF16)]]
            PLAST = [small[:, 160 + 8 * i:168 + 8 * i] for i in range(2)]
            bpl = [buf("plast0"), buf("plast1")]
            A_SB = ta(5120, 512, BF16); Sst = ta(5376, 128); S_BF = ta(5504, 1024, BF16)
            OSQ = ta(6016, 512, BF16); RT = ta(6272, 512); ON = ta(6784, 512); THOG = ta(7296, 512); THGB = ta(7808, 512)
            GU = ta(9856, 512, BF16); THGA = ta(8832, 512); ZV = ta(9344, 512)
            dve(lambda h: h.memset(D1.ap, 0.0), [], D1.bufs)
            for sl_ in range(2):
                dve(lambda h, sl_=sl_: h.memset(KHM[sl_][0].ap[64:128, :], 0.0), [], KHM[sl_][0].bufs)
                dve(lambda h, sl_=sl_: h.memset(KHM[sl_][1].ap[0:64, :], 0.0), [], KHM[sl_][1].bufs)
            OFF = dict(u=0, ga=128, q=256, f=384, i=512, og=640, gb=768)

            def wslot(h):
                return wa(((h + 1) % 2) * 8192, 7168)

            def proj(h, tb, name, wsl):
                w3_ = wsl.ap.rearrange("p (k n) -> p k n", n=896)
                pb = bank()
                o = OFF[name]
                for kc in range(8):
                    mm(pb.ap, w3_[:, kc, o:o + 128], hT3[:, kc, tb * 512:(tb + 1) * 512], kc == 0, kc == 7,
                       wsl.bufs + hTb[tb * 4:tb * 4 + 4], pb.bufs)
                return pb

            def stage1(h, tb, sl):
                wsl = wslot(h)
                pbf = proj(h, tb, "f", wsl)
                act(F_.ap, pbf.ap, AF.Tanh, pbf.bufs, F_.bufs, scale=0.5)
                pbq = proj(h, tb, "q", wsl)
                act(THQ.ap, pbq.ap, AF.Tanh, pbq.bufs, THQ.bufs, scale=0.5)
                pbi = proj(h, tb, "i", wsl)
                act(IT.ap, pbi.ap, AF.Copy, pbi.bufs, IT.bufs)
                act(F_.ap, F_.ap, AF.Identity, F_.bufs + [bsmall], F_.bufs, scale=FCA[:, h:h + 1], bias=FCB[:, h:h + 1])
                f3 = F_.ap.rearrange("p (c j) -> p c j", j=64)
                d3 = D1.ap.rearrange("p (c j) -> p c j", j=64)
                p3 = Pc.ap.rearrange("p (c j) -> p c j", j=64)
                act(d3[:, :, 0:1], f3[:, :, 0:1], AF.Copy, F_.bufs, D1.bufs)
                dve(lambda hh: hh.tensor_tensor_scan(out=Pc.ap, data0=F_.ap, data1=D1.ap, initial=1.0, op0=ALU.mult, op1=ALU.max),
                    F_.bufs + D1.bufs, Pc.bufs)
                act(PLAST[sl], p3[:, :, 63], AF.Copy, Pc.bufs, [bpl[sl]])
                stt(THQ.ap, THQ.ap, 1.0, pbq.ap, ALU.add, ALU.mult, THQ.bufs + pbq.bufs, THQ.bufs)
                pool(lambda hh: hh.tensor_scalar(out=F_.ap, in0=F_.ap, scalar1=-1.0, scalar2=1.0, op0=ALU.mult, op1=ALU.add),
                     F_.bufs, F_.bufs)
                dve(lambda hh: hh.reciprocal(out=RP.ap, in_=Pc.ap), Pc.bufs, RP.bufs)
                pool(lambda hh: hh.tensor_tensor(out=KP[sl].ap, in0=F_.ap, in1=RP.ap, op=ALU.mult),
                     RP.bufs + F_.bufs, KP[sl].bufs)
                pool(lambda hh: hh.tensor_tensor(out=KH.ap.rearrange("p (c j) -> p c j", j=64),
                                                 in0=KP[sl].ap.rearrange("p (c j) -> p c j", j=64),
                                                 in1=bc_last(PLAST[sl], 64), op=ALU.mult),
                     KP[sl].bufs + [bpl[sl]], KH.bufs)
                pool(lambda hh: hh.tensor_tensor(out=QH[sl].ap, in0=THQ.ap, in1=Pc.ap, op=ALU.mult),
                     THQ.bufs + Pc.bufs, QH[sl].bufs)
            def stage1b(h, tb, sl):
                pbt = bank()
                pbtb = pbt.ap.bitcast(BF16)
                for jj in range(4):
                    P.c("pe", lambda hh, jj=jj: hh.transpose(pbtb[:, jj * 128:(jj + 1) * 128], IT.ap[:, jj * 128:(jj + 1) * 128], ident_b),
                        r=IT.bufs + [B["cstb"]], w=pbt.bufs, mode=("t", 128, 128))
                for jj in range(4):
                    P.c("pe", lambda hh, jj=jj: hh.transpose(pbtb[:, 512 + jj * 128:512 + (jj + 1) * 128],
                                                             KH.ap[:, jj * 128:(jj + 1) * 128], ident_b),
                        r=KH.bufs + [B["cstb"]], w=pbt.bufs, mode=("t", 128, 128))
                act(ITOK[sl].ap, pbtb[:, 0:512], AF.Copy, pbt.bufs, ITOK[sl].bufs)
                act(KHM[sl][0].ap[0:64, :], pbtb[0:64, 512:1024], AF.Copy, pbt.bufs, KHM[sl][0].bufs)
                act(KHM[sl][1].ap[64:128, :], pbtb[64:128, 512:1024], AF.Copy, pbt.bufs, KHM[sl][1].bufs)

            def stage2(h, tb, sl):
                if STOP == "b":
                    return
                wsl = wslot(h)
                pbs = bank()
                for jj in range(4):
                    c0 = jj * 128
                    mm(pbs.ap[:, c0:c0 + 128], KP[sl].ap[:, c0:c0 + 128], QH[sl].ap[:, c0:c0 + 128], True, True,
                       KP[sl].bufs + QH[sl].bufs, pbs.bufs)
                tt(A_SB.ap.rearrange("p (a t) -> p a t", t=128), pbs.ap.rearrange("p (a t) -> p a t", t=128), bc_mid(mask_bd, 4),
                   ALU.mult, pbs.bufs + [B["cst"]], A_SB.bufs)
                if STOP == "c":
                    return
                pbus = [bank(), bank()]
                def ureg(cc):
                    return pbus[cc // 4], pbus[cc // 4].ap[:, (cc % 4) * 128:(cc % 4 + 1) * 128]
                for cc in range(8):
                    jj, hh_ = cc // 2, cc % 2
                    mm(ureg(cc)[1], KHM[sl][hh_].ap[:, jj * 128:(jj + 1) * 128],
                       ITOK[sl].ap[:, jj * 128:(jj + 1) * 128], True, True,
                       KHM[sl][hh_].bufs + ITOK[sl].bufs, ureg(cc)[0].bufs)
                SS = [Sst, Reg(QFt[:, 0:128], [buf("QF")])]
                if tb == 0:
                    dve(lambda hh: hh.memset(SS[0].ap, 0.0), [], SS[0].bufs)
                for cc in range(8):
                    cur, nx = SS[cc % 2], SS[(cc + 1) % 2]
                    act(S_BF.ap[:, cc * 128:(cc + 1) * 128], cur.ap, AF.Copy, cur.bufs, [buf("sbf%d" % cc)])
                    stt(nx.ap, cur.ap, PLAST[sl][:, cc:cc + 1], ureg(cc)[1], ALU.mult, ALU.add,
                        cur.bufs + [bpl[sl]] + ureg(cc)[0].bufs, nx.bufs)
                pbog = proj(h, tb, "og", wsl)
                act(THOG.ap, pbog.ap, AF.Tanh, pbog.bufs, THOG.bufs, scale=0.5)
                pbgb = proj(h, tb, "gb", wsl)
                act(THGB.ap, pbgb.ap, AF.Tanh, pbgb.bufs, THGB.bufs, scale=0.5)
                stt(THOG.ap, THOG.ap, 1.0, pbog.ap, ALU.add, ALU.mult, THOG.bufs + pbog.bufs, THOG.bufs)
                stt(THGB.ap, THGB.ap, 1.0, THOG.ap, ALU.add, ALU.mult, THGB.bufs + THOG.bufs, THGB.bufs)
                pbuu = proj(h, tb, "u", wsl)
                act(GU.ap, pbuu.ap, AF.Gelu_apprx_tanh, pbuu.bufs, GU.bufs)
                pbga = proj(h, tb, "ga", wsl)
                act(THGA.ap, pbga.ap, AF.Tanh, pbga.bufs, THGA.bufs, scale=0.5)
                pbz = bank()
                for jj in range(4):
                    j = tb * 4 + jj
                    mm(pbz.ap[:, jj * 128:(jj + 1) * 128], vn[j].ap[:, h * 128:(h + 1) * 128], wcT[:, h * 128:(h + 1) * 128],
                       True, True, vn[j].bufs + [bconst], pbz.bufs)
                stt(ZV.ap.rearrange("p (a t) -> p a t", t=128), pbz.ap.rearrange("p (a t) -> p a t", t=128), VF[:, V_LNG, h:h + 1],
                    bc_mid(Rsb[:, h * 128:(h + 1) * 128], 4), ALU.mult, ALU.add, pbz.bufs + [bvec, bconst], ZV.bufs)
                stt(THGA.ap, THGA.ap, 1.0, GU.ap, ALU.add, ALU.mult, THGA.bufs + GU.bufs, THGA.bufs)
                stt(ZV.ap, THGA.ap, 0.5, ZV.ap, ALU.mult, ALU.mult, THGA.bufs + ZV.bufs, ZV.bufs)
            def stage2b(h, tb, sl):
                pbo = bank()
                for jj in range(4):
                    c0 = jj * 128
                    mm(pbo.ap[:, c0:c0 + 128], ITOK[sl].ap[:, c0:c0 + 128], A_SB.ap[:, c0:c0 + 128], True, False,
                       ITOK[sl].bufs + A_SB.bufs, pbo.bufs)
                    for cc in (2 * jj, 2 * jj + 1):
                        c1 = cc * 64
                        P.c("pe", lambda hh, c1=c1, cc=cc: hh.matmul(pbo.ap[:, c1:c1 + 64], S_BF.ap[:, cc * 128:(cc + 1) * 128],
                                                                      QH[sl].ap[:, c1:c1 + 64], start=False, stop=(cc % 2 == 1)),
                            r=[buf("sbf%d" % cc)] + QH[sl].bufs, w=pbo.bufs, mode=pmode(S_BF.ap[:, 0:128]))
                act(OSQ.ap, pbo.ap, AF.Square, pbo.bufs, OSQ.bufs)
                pbn = bank()
                mm(pbn.ap, ones_b, OSQ.ap, True, True, OSQ.bufs + [B["cstb"]], pbn.bufs)
                act(RT.ap, pbn.ap, AF.Ln, pbn.bufs + [bsmall], RT.bufs, bias=EPS6X4, scale=1.0 / 128)
                act(RT.ap, RT.ap, AF.Exp, RT.bufs, RT.bufs, scale=-0.5)
                pool(lambda hh: hh.tensor_tensor(out=THGB.ap, in0=THGB.ap, in1=RT.ap, op=ALU.mult), THGB.bufs + RT.bufs, THGB.bufs)
                stt(ON.ap, pbo.ap, GQ[:, h:h + 1], THGB.ap, ALU.mult, ALU.mult, pbo.bufs + THGB.bufs + [bsmall], ON.bufs)
                yreg = xa(YT0 + h * S + tb * 512, 512)
                pool(lambda hh: hh.tensor_tensor(out=yreg.ap, in0=ZV.ap, in1=ON.ap, op=ALU.add), ZV.bufs + ON.bufs, yreg.bufs)

            def load_wfm(h):
                wsl = wslot(h)
                ld("pool", wsl.ap.rearrange("p (k n) -> p k n", n=896), wfm_d[h].rearrange("(k p) n -> p k n", p=128),
                   wsl.bufs[0], wsl.bufs)

            wos = wa(8192, 8192)
            wo3 = wos.ap.rearrange("p (k n) -> p k n", n=1024)
            its = [(h, tb) for h in range(8) for tb in range(NB)]
            load_wfm(1)
            stage1(its[0][0], its[0][1], 0)
            stage1b(its[0][0], its[0][1], 0)
            for k in range(len(its)):
                nxt = k + 1 < len(its)
                if nxt:
                    stage1(its[k + 1][0], its[k + 1][1], (k + 1) % 2)
                stage2(its[k][0], its[k][1], k % 2)
                if nxt:
                    stage1b(its[k + 1][0], its[k + 1][1], (k + 1) % 2)
                stage2b(its[k][0], its[k][1], k % 2)
                if its[k][1] == NB - 1:
                    if its[k][0] + 2 < 8:
                        load_wfm(its[k][0] + 2)
                    elif its[k][0] == 6:
                        ld("pool", wo3, wout_d.rearrange("(k p) n -> p k n", p=128), wos.bufs[0], wos.bufs)
            if upto <= 2:
                break

            make_gtbc(0, s)
            XR = [ta(0, 1024), ta(1024, 1024)]
            X1 = [ta(2048, 1024), ta(3072, 1024)]
            junk3 = ta(4096, 1024, BF16)
            XN = [ta(4608, 1024, BF16), ta(5120, 1024, BF16)]
            sq2 = small[:, 176:176 + 2 * NT]
            lg3 = lg[:, :].rearrange("p (t n) -> p t n", n=20)
            bx1 = buf("x1s%d" % s)
            yT3 = XA[:, YT0:YT0 + 8 * S].rearrange("p (k s) -> p k s", s=S)
            def p3a(j):
                pb2 = bank2()
                for half in range(2):
                    for kc in range(8):
                        yr = xa(YT0 + kc * S + j * 128, 128)
                        mm(pb2.ap[:, half * 512:(half + 1) * 512], yT3[:, kc, j * 128:(j + 1) * 128],
                           wo3[:, kc, half * 512:(half + 1) * 512], kc == 0, kc == 7, yr.bufs + wos.bufs, pb2.bufs)
                xr = XR[j % 2]
                ld("sp", xr.ap, x_d[s, j * 128:(j + 1) * 128, :], xr.bufs[0], xr.bufs)
                return pb2

            def p3b(j, pb2):
                xr = XR[j % 2]
                r2c = sq2[:, 2 * j:2 * j + 1]
                r3c = sq2[:, 2 * j + 1:2 * j + 2]
                act(junk3.ap, pb2.ap, AF.Square, pb2.bufs, junk3.bufs + [bsmall], accum_out=r2c)
                act(r2c, r2c, AF.Ln, [bsmall], [bsmall], bias=EPS6, scale=1.0 / D)
                act(r2c, r2c, AF.Exp, [bsmall], [bsmall], scale=-0.5)
                x1 = X1[j % 2]
                stt(x1.ap, pb2.ap, r2c, gtbc[:, 0:D], ALU.mult, ALU.mult, pb2.bufs + [bsmall, bgt[0]], x1.bufs)
                tt(x1.ap, x1.ap, xr.ap, ALU.add, x1.bufs + xr.bufs, x1.bufs)
                P.dma("sp", lambda h, x1=x1, j=j, s=s: h.dma_start(out=x1s_d[s, j * 128:(j + 1) * 128, :], in_=x1.ap),
                      bx1, r=x1.bufs, w=[bx1])
                act(junk3.ap, x1.ap, AF.Square, x1.bufs, junk3.bufs + [bsmall], accum_out=r3c)
                act(r3c, r3c, AF.Ln, [bsmall], [bsmall], bias=EPS6, scale=1.0 / D)
                act(r3c, r3c, AF.Exp, [bsmall], [bsmall], scale=-0.5)
                xb = XN[j % 2]
                act(xb.ap, x1.ap, AF.Copy, x1.bufs + [bsmall], xb.bufs, scale=r3c)

            def p3c(j):
                xb = XN[j % 2]
                pb = bank()
                pbb = pb.ap.bitcast(BF16)
                for kc in range(8):
                    P.c("pe", lambda h, pbb=pbb, xb=xb, kc=kc: h.transpose(pbb[:, kc * 128:(kc + 1) * 128],
                                                                            xb.ap[:, kc * 128:(kc + 1) * 128], ident_b),
                        r=xb.bufs + [B["cstb"]], w=pb.bufs, mode=("t", 128, 128))
                for kc in range(8):
                    ts(hT3[:, kc, j * 128:(j + 1) * 128], pbb[:, kc * 128:(kc + 1) * 128],
                       scl4[:, 2, kc, s:s + 1], scl4[:, 3, kc, s:s + 1], ALU.mult, ALU.add, pb.bufs + [bscl], [hTb[j]])
                pbr = bank()
                for kc in range(8):
                    mm(pbr.ap[:, 0:20], hT3[:, kc, j * 128:(j + 1) * 128], wrb[:, kc * 20:(kc + 1) * 20], kc == 0, kc == 7,
                       [hTb[j], B["wrb"]], pbr.bufs)
                tt(lg3[:, j, :], pbr.ap[:, 0:20], brbc[:, :], ALU.add, pbr.bufs + [B["brbc"]], [blg])

            pcur = p3a(0)
            for j in range(NT):
                pnext = p3a(j + 1) if j + 1 < NT else None
                p3b(j, pcur)
                p3c(j)
                pcur = pnext
            if upto <= 3:
                break

            RTm = rtmp[:, :]
            def rt(off, n):
                return RTm[:, off * NT:(off + n) * NT]
            G = lg3[:, :, 0:4]
            E4 = lg3[:, :, 4:20].rearrange("p t (g e) -> p t g e", e=4)
            gmax = rt(0, 1); ohg = rt(1, 4).rearrange("p (t g) -> p t g", g=4); dG = rt(5, 4).rearrange("p (t g) -> p t g", g=4)
            sumg = rt(9, 1); EM = rt(10, 16).rearrange("p (t g e) -> p t g e", g=4, e=4)
            El = rt(26, 4).rearrange("p (t e) -> p t e", e=4); m1 = rt(30, 1); oh1 = rt(31, 4).rearrange("p (t e) -> p t e", e=4)
            El2 = rt(35, 4).rearrange("p (t e) -> p t e", e=4); m2 = rt(39, 1); oh2 = rt(40, 4).rearrange("p (t e) -> p t e", e=4)
            rr = rt(44, 1); wa_ = rt(45, 1); wb_ = rt(46, 1); ce = rt(47, 4).rearrange("p (t e) -> p t e", e=4)
            ce2 = rt(51, 4).rearrange("p (t e) -> p t e", e=4)
            comb4 = comb[:, :].rearrange("p (t g e) -> p t g e", g=4, e=4)
            RB = [brt]
            dve(lambda h: h.tensor_reduce(out=gmax, in_=G, axis=AX.X, op=ALU.max), [blg], RB)
            tt(ohg, G, bc_last(gmax, 4), ALU.is_equal, [blg] + RB, RB)
            tt(dG, G, bc_last(gmax, 4), ALU.subtract, [blg] + RB, RB)
            act(dG, dG, AF.Exp, RB, RB)
            dve(lambda h: h.tensor_reduce(out=sumg, in_=dG, axis=AX.X, op=ALU.add), RB, RB)
            dve(lambda h: h.reciprocal(out=sumg, in_=sumg), RB, RB)
            tt(EM, E4, ohg.unsqueeze(3).to_broadcast([128, NT, 4, 4]), ALU.mult, [blg] + RB, RB)
            dve(lambda h: h.tensor_reduce(out=El, in_=EM.rearrange("p t g e -> p t e g"), axis=AX.X, op=ALU.add), RB, RB)
            dve(lambda h: h.tensor_reduce(out=m1, in_=El, axis=AX.X, op=ALU.max), RB, RB)
            tt(oh1, El, bc_last(m1, 4), ALU.is_equal, RB, RB)
            stt(El2, oh1, -1e30, El, ALU.mult, ALU.add, RB, RB)
            dve(lambda h: h.tensor_reduce(out=m2, in_=El2, axis=AX.X, op=ALU.max), RB, RB)
            tt(oh2, El2, bc_last(m2, 4), ALU.is_equal, RB, RB)
            tt(rr, m2, m1, ALU.subtract, RB, RB)
            act(rr, rr, AF.Exp, RB, RB)
            ts(wa_, rr, 1.0, None, ALU.add, None, RB, RB)
            dve(lambda h: h.reciprocal(out=wa_, in_=wa_), RB, RB)
            tt(wa_, wa_, sumg, ALU.mult, RB, RB)
            tt(wb_, wa_, rr, ALU.mult, RB, RB)
            tt(ce, oh1, bc_last(wa_, 4), ALU.mult, RB, RB)
            tt(ce2, oh2, bc_last(wb_, 4), ALU.mult, RB, RB)
            tt(ce, ce, ce2, ALU.add, RB, RB)
            dve(lambda h: h.tensor_copy(out=comb4, in_=ce.unsqueeze(2).to_broadcast([128, NT, 4, 4])), RB, [bcomb])
            tt(comb4, comb4, ohg.unsqueeze(3).to_broadcast([128, NT, 4, 4]), ALU.mult, RB + [bcomb], [bcomb])
            comb3 = comb[:, :].rearrange("p (t e) -> p t e", e=16)

            AT = [ta(0, 2048, BF16), ta(1024, 2048, BF16)]
            S1 = [ta(2048, 512, BF16), ta(2304, 512, BF16)]
            def moe_w(e):
                sl = e % 2
                w1s = wa(sl * 8192, 4096); w3s = wa(sl * 8192 + 4096, 4096); w2s = wa(16384, 4096)
                return (w1s, w3s, w2s, w1s.ap.rearrange("p (k n) -> p k n", n=512), w3s.ap.rearrange("p (k n) -> p k n", n=512),
                        w2s.ap.rearrange("p (k n) -> p k n", n=1024))

            def moe_ld13(e):
                w1s, w3s, w2s, w1v, w3v, w2v = moe_w(e)
                ld("pool", w1v, w1_d[e].rearrange("(k p) n -> p k n", p=128), w1s.bufs[0], w1s.bufs)
                ld("pool", w3v, w3_d[e].rearrange("(k p) n -> p k n", p=128), w3s.bufs[0], w3s.bufs)

            def moe_ld2(e):
                w1s, w3s, w2s, w1v, w3v, w2v = moe_w(e)
                ld("pool", w2v, w2_d[e].rearrange("(k p) n -> p k n", p=128), w2s.bufs[0], w2s.bufs)

            moe_ld13(0)
            moe_ld2(0)
            for e in range(NE):
                w1s, w3s, w2s, w1v, w3v, w2v = moe_w(e)
                if e + 1 < NE:
                    moe_ld13(e + 1)
                for tb in range(NB):
                    at = AT[tb % 2]
                    at3 = at.ap.rearrange("p (f t) -> p f t", t=512)
                    for fc in range(4):
                        p1 = bank()
                        for kc in range(8):
                            mm(p1.ap, w1v[:, kc, fc * 128:(fc + 1) * 128], hT3[:, kc, tb * 512:(tb + 1) * 512], kc == 0, kc == 7,
                               w1s.bufs + hTb[tb * 4:tb * 4 + 4], p1.bufs)
                        p3 = bank()
                        for kc in range(8):
                            mm(p3.ap, w3v[:, kc, fc * 128:(fc + 1) * 128], hT3[:, kc, tb * 512:(tb + 1) * 512], kc == 0, kc == 7,
                               w3s.bufs + hTb[tb * 4:tb * 4 + 4], p3.bufs)
                        s1t = S1[fc % 2]
                        act(s1t.ap, p1.ap, AF.Silu, p1.bufs, s1t.bufs)
                        tt(at3[:, fc, :], s1t.ap, p3.ap, ALU.mult, s1t.bufs + p3.bufs, at.bufs)
                    for jj in range(4):
                        j = tb * 4 + jj
                        py = bank2()
                        for half in range(2):
                            for fc in range(4):
                                mm(py.ap[:, half * 512:(half + 1) * 512], at3[:, fc, jj * 128:(jj + 1) * 128],
                                   w2v[:, fc, half * 512:(half + 1) * 512], fc == 0, fc == 3, at.bufs + w2s.bufs, py.bufs)
                        acc = xacc(j)
                        if e == 0:
                            ts(acc.ap, py.ap, comb3[:, j, e:e + 1], None, ALU.mult, None, py.bufs + [bcomb], acc.bufs)
                        else:
                            stt(acc.ap, py.ap, comb3[:, j, e:e + 1], acc.ap, ALU.mult, ALU.add, py.bufs + [bcomb] + acc.bufs, acc.bufs)
                if e + 1 < NE:
                    moe_ld2(e + 1)

            if s == 0:
                prefetch_mixer_weights()
                phase1(1)
            make_gtbc(1, s)
            sq4 = small[:, 216:216 + NT]
            junk5 = ta(4096, 1024, BF16)
            for j in range(NT):
                act(junk5.ap, xacc(j).ap, AF.Square, xacc(j).bufs, junk5.bufs + [bsmall], accum_out=sq4[:, j:j + 1])
            act(sq4, sq4, AF.Sqrt, [bsmall], [bsmall], bias=EPS6, scale=1.0 / D)
            dve(lambda h: h.reciprocal(out=sq4, in_=sq4), [bsmall], [bsmall])
            X5 = [ta(5120, 1024), ta(6144, 1024)]
            O5 = [ta(7168, 1024), ta(8192, 1024)]
            bout = buf("out%d" % s)
            for j in range(NT):
                x5 = X5[j % 2]
                P.dma("sp", lambda h, x5=x5, j=j, s=s: h.dma_start(out=x5.ap, in_=x1s_d[s, j * 128:(j + 1) * 128, :]),
                      x5.bufs[0], r=[bx1], w=x5.bufs)
                o5 = O5[j % 2]
                stt(o5.ap, xacc(j).ap, sq4[:, j:j + 1], gtbc[:, D:2 * D], ALU.mult, ALU.mult, xacc(j).bufs + [bsmall, bgt[1]], o5.bufs)
                tt(o5.ap, o5.ap, x5.ap, ALU.add, o5.bufs + x5.bufs, o5.bufs)
                P.dma("sp", lambda h, o5=o5, j=j, s=s: h.dma_start(out=out_d[s, j * 128:(j + 1) * 128, :], in_=o5.ap),
                      bout, r=o5.bufs, w=[bout], is_out=True)

        if debug:
            P.dma("sp", lambda h: h.dma_start(out=dbg["hT"], in_=hT[:, :]), buf("dbg_hT"), r=hTb, w=[B["dbg_hT"]], is_out=True)
            if upto in (2, 3):
              P.dma("sp", lambda h: h.dma_start(out=dbg["yT"], in_=XA[:, NT * 1024:NT * 1024 + 8 * S]), buf("dbg_yT"), r=XAb, w=[B["dbg_yT"]], is_out=True)
            P.dma("sp", lambda h: h.dma_start(out=dbg["misc"][:, 0:96], in_=adaT[:, :]), buf("dbg_m"), r=[badaT], w=[B["dbg_m"]], is_out=True)
            P.dma("sp", lambda h: h.dma_start(out=dbg["misc"][:, 128:224], in_=scl[:, :]), B["dbg_m"], r=[bscl], w=[B["dbg_m"]], is_out=True)
            if upto >= 3:
              P.dma("sp", lambda h: h.dma_start(out=dbg["misc"][:, 256:256 + NT * 20], in_=lg[:, :]), B["dbg_m"], r=[blg], w=[B["dbg_m"]], is_out=True)
            if upto >= 4:
              P.dma("sp", lambda h: h.dma_start(out=dbg["misc"][:, 1024:1024 + NT * 16], in_=comb[:, :]), B["dbg_m"], r=[bcomb], w=[B["dbg_m"]], is_out=True)
            P.dma("sp", lambda h: h.dma_start(out=dbg["misc"][:, 2048:3072], in_=Rsb[:, :]), B["dbg_m"], r=[bconst], w=[B["dbg_m"]], is_out=True)

        P.lower(nc)
    return nc


def host_prep(inputs, S=2048):
    f = lambda a: np.ascontiguousarray(np.asarray(a, dtype=np.float32))
    w_in = f(inputs["w_in"][0])
    sl = [w_in[:, i * D:(i + 1) * D] for i in range(8)]
    u, v, q, ff_, i_, og, ga, gb = sl
    wfm = np.stack([np.concatenate([m[:, h * 128:(h + 1) * 128] for m in (u, ga, q, ff_, i_, og, gb)], axis=1)
                    for h in range(8)], axis=0)
    fm = lambda vec: f(vec).reshape(8, 128).T
    lb = f(inputs["lb_logits"])
    vecfm = np.zeros((128, 8, 8), np.float32)
    for k, vv in enumerate([inputs["g_pre_mix"][0], inputs["ln_v_g"][0], lb[0], lb[1], inputs["g_hgrn_norm"][0],
                            inputs["g_pre_ffn"][0], inputs["g_post_mix"][0], inputs["g_post_ffn"][0]]):
        vecfm[:, k, :] = fm(vv)
    cst = np.zeros((128, 1024), np.float32)
    cst[:, 0:128] = np.eye(128)
    cst[:, 128:256] = 1.0
    sidx = np.arange(128)[:, None]
    tidx = np.arange(128)[None, :]
    cst[:, 256:384] = ((sidx <= tidx) & (sidx // 64 == tidx // 64)).astype(np.float32)
    cst[:, 384:512] = (sidx <= tidx).astype(np.float32)
    cst[0, 512:640] = 1.0
    cst[1, 640:768] = 1.0
    shared = {
        "w_ada": f(inputs["w_ada"][0]),
        "b_ada2": np.ascontiguousarray(np.broadcast_to(f(inputs["b_ada"][0])[None, :], (2, 6 * D))),
        "vecfm": vecfm,
        "g_post_mix_bc": np.ascontiguousarray(np.broadcast_to(f(inputs["g_post_mix"][0])[None, :], (128, D))),
        "g_post_ffn_bc": np.ascontiguousarray(np.broadcast_to(f(inputs["g_post_ffn"][0])[None, :], (128, D))),
        "w_fm": np.ascontiguousarray(wfm),
        "w_v": np.ascontiguousarray(v),
        "w_spT": np.ascontiguousarray(np.transpose(f(inputs["w_spatial"][0]), (2, 0, 1))),
        "lnb2": np.ascontiguousarray(np.stack([f(inputs["ln_v_b"][0]), np.ones(D, np.float32)], axis=0)),
        "b_sp": f(inputs["b_spatial"][0]).reshape(1, 1024),
        "w_out": f(inputs["w_out"][0]),
        "w_r": np.ascontiguousarray(np.concatenate([f(inputs["w_router_group"][0]), f(inputs["w_router_expert"][0])], axis=1)),
        "b_r_bc": np.ascontiguousarray(np.broadcast_to(
            np.concatenate([f(inputs["b_router_group"][0]), f(inputs["b_router_expert"][0])])[None, :], (128, 20))),
        "w1": f(inputs["w1"][0]), "w3": f(inputs["w3"][0]), "w2": f(inputs["w2"][0]),
        "cst": cst,
    }
    x = f(inputs["x"])
    c = f(inputs["c"])
    ncore = x.shape[0] // 2
    maps = []
    for k in range(ncore):
        m = dict(shared)
        m["x"] = np.ascontiguousarray(x[2 * k:2 * k + 2, :S])
        cc = c[2 * k:2 * k + 2]
        m["cT"] = np.ascontiguousarray(cc.reshape(2, 8, 128).transpose(2, 1, 0))
        maps.append(m)
    return maps


def kernel(**inputs):
    maps = host_prep(inputs)
    nc = build()
    res = run_bass_kernel_spmd(nc, maps, core_ids=list(range(NCORES)))
    return np.concatenate([np.asarray(r["out"]) for r in res.results], axis=0).astype(np.float32)
```

```python
import contextlib
import numpy as np
import concourse.bass as bass
import concourse.mybir as mybir
from concourse.bass_utils import run_bass_kernel_spmd

F32 = mybir.dt.float32
BF16 = mybir.dt.bfloat16
AF = mybir.ActivationFunctionType
ALU = mybir.AluOpType
AX = mybir.AxisListType

D = 1024
NCORES = 8
NE = 16
FF = 512
ENGS = ("pe", "act", "dve", "pool", "sp")
STOP = None


class Buf:
    __slots__ = ("name", "writers", "readers", "sem_idx", "dcount")

    def __init__(self, name):
        self.name = name
        self.writers = {}
        self.readers = {}
        self.sem_idx = None
        self.dcount = 0


class Op:
    __slots__ = ("id", "eng", "fn", "deps", "kind", "val", "dsem", "dval")


class Plan:
    def __init__(self):
        self.ops = []
        self.n_dma_sems = 0
        self.out_ops = []

    def _mk(self, eng, fn, kind):
        op = Op()
        op.id = len(self.ops)
        op.eng = eng
        op.fn = fn
        op.kind = kind
        op.deps = []
        op.val = None
        op.dsem = None
        op.dval = None
        self.ops.append(op)
        return op

    @staticmethod
    def _key(op):
        return ("d", op.dsem) if op.kind == "d" else ("c", op.eng)

    def _track(self, op, r, w):
        deps = {}
        for b in r:
            for o in b.writers.values():
                deps[o.id] = o
        for b in w:
            for o in b.writers.values():
                deps[o.id] = o
            for o in b.readers.values():
                deps[o.id] = o
        op.deps = [o for o in deps.values() if not (o.kind == "c" and o.eng == "pe" and op.kind == "c" and op.eng == "pe")]
        k = self._key(op)
        for b in r:
            b.readers[k] = op
        for b in w:
            b.writers[k] = op
            b.readers = {}

    def c(self, eng, fn, r=(), w=(), mode=None):
        op = self._mk(eng, fn, "c")
        self._track(op, r, w)
        if eng == "pe":
            last = getattr(self, "_last_pe", None)
            if last is not None and last[1] != mode and all(d.id != last[0].id for d in op.deps):
                op.deps.append(last[0])
            self._last_pe = (op, mode)
        return op

    def dma(self, eng, fn, dst, r=(), w=(), is_out=False):
        op = self._mk(eng, fn, "d")
        if dst.sem_idx is None:
            dst.sem_idx = self.n_dma_sems
            self.n_dma_sems += 1
        dst.dcount += 16
        op.dsem = dst.sem_idx
        op.dval = dst.dcount
        self._track(op, r, w)
        if is_out:
            self.out_ops.append(op)
        return op

    def lower(self, nc):
        ops = self.ops
        needed = set()
        for op in ops:
            for d in op.deps:
                if d.kind == "c":
                    needed.add(d.id)
        cnt = {e: 0 for e in ENGS}
        for op in ops:
            if op.kind == "c" and op.id in needed:
                cnt[op.eng] += 1
                op.val = cnt[op.eng]
        per_eng = {e: [o for o in ops if o.eng == e] for e in ENGS}
        out_ops = self.out_ops
        with contextlib.ExitStack() as st:
            esem = {e: st.enter_context(nc.semaphore("s_" + e)) for e in ENGS}
            dsem = [st.enter_context(nc.semaphore("d%d" % i)) for i in range(self.n_dma_sems)]
            block = st.enter_context(nc.Block())

            def run(h, e):
                waited = {}

                def wait_for(d):
                    if d.kind == "c":
                        key, sem, val = ("c", d.eng), esem[d.eng], d.val
                    else:
                        key, sem, val = ("d", d.dsem), dsem[d.dsem], d.dval
                    if waited.get(key, 0) < val:
                        h.wait_ge(sem, val)
                        waited[key] = val

                for op in per_eng[e]:
                    for d in op.deps:
                        wait_for(d)
                    inst = op.fn(h)
                    if op.kind == "d":
                        inst.then_inc(dsem[op.dsem], 16)
                    elif op.id in needed:
                        inst.then_inc(esem[e], 1)
                if e == "sp":
                    for d in out_ops:
                        wait_for(d)

            @block.tensor
            def _(h):
                run(h, "pe")

            @block.scalar
            def _(h):
                run(h, "act")

            @block.vector
            def _(h):
                run(h, "dve")

            @block.gpsimd
            def _(h):
                run(h, "pool")

            @block.sync
            def _(h):
                run(h, "sp")


class Reg:
    __slots__ = ("ap", "bufs")

    def __init__(self, ap, bufs):
        self.ap = ap
        self.bufs = list(bufs)


def build(S=2048, upto=9, debug=False):
    NT = S // 128
    NB = S // 512
    nc = bass.Bass("TRN2", target_bir_lowering=False)
    P = Plan()

    def din(name, shape, dt=F32):
        return nc.dram_tensor(name, list(shape), dt, kind="ExternalInput").ap()

    x_d = din("x", [2, S, D])
    cT_d = din("cT", [128, 8, 2])
    wada_d = din("w_ada", [D, 6 * D])
    bada_d = din("b_ada2", [2, 6 * D])
    vecfm_d = din("vecfm", [128, 8, 8])
    gpm_d = din("g_post_mix_bc", [128, D])
    gpf_d = din("g_post_ffn_bc", [128, D])
    wfm_d = din("w_fm", [8, D, 896])
    wv_d = din("w_v", [D, D])
    wspT_d = din("w_spT", [128, 8, 128])
    lnb2_d = din("lnb2", [2, D])
    bsp_d = din("b_sp", [1, 8 * 128])
    wout_d = din("w_out", [D, D])
    wr_d = din("w_r", [D, 20])
    brbc_d = din("b_r_bc", [128, 20])
    w1_d = din("w1", [NE, D, FF])
    w3_d = din("w3", [NE, D, FF])
    w2_d = din("w2", [NE, FF, D])
    cst_d = din("cst", [128, 1024])
    out_d = nc.dram_tensor("out", [2, S, D], F32, kind="ExternalOutput").ap()
    x1s_d = nc.dram_tensor("x1s", [2, S, D], F32, kind="Internal").ap()
    dbg = {}
    if debug:
        dbg["hT"] = nc.dram_tensor("dbg_hT", [128, 8 * S], BF16, kind="ExternalOutput").ap()
        dbg["yT"] = nc.dram_tensor("dbg_yT", [128, 8 * S], BF16, kind="ExternalOutput").ap()
        dbg["misc"] = nc.dram_tensor("dbg_misc", [128, 4096], F32, kind="ExternalOutput").ap()

    with contextlib.ExitStack() as st:
        def sb(name, cols, dt):
            return st.enter_context(nc.sbuf_tensor("sb_" + name, [128, cols], dt))

        cst = sb("cst", 768, F32)
        cstb = sb("cstb", 256, BF16)
        vecfm = sb("vecfm", 64, F32)
        brbc = sb("brbc", 20, F32)
        small = sb("small", 256, F32)
        adaT = sb("adaT", 96, F32)
        scl = sb("scl", 96, F32)
        gtbc = sb("gtbc", 2 * D, F32)
        wcT = sb("wcT", 1024, BF16)
        Rsb = sb("Rsb", 1024, F32)
        wrb = sb("wrb", 160, BF16)
        lg = sb("lg", NT * 20, F32)
        comb = sb("comb", NT * 16, F32)
        rtmp = sb("rtmp", NT * 64, F32)
        hT = sb("hT", 8 * S, BF16)
        XA = sb("XA", 16 * S, BF16)
        WA = sb("WA", 20480, BF16)
        TA = sb("TA", 10240, F32)
        QFt = sb("QF", 512, F32)
        OFt = sb("OF", 512, F32)
        psum = [st.enter_context(nc.psum_tensor("ps%d" % i, [128, 1024], F32)) for i in range(4)]

        B = {}

        def buf(name):
            if name not in B:
                B[name] = Buf(name)
            return B[name]

        bank_bufs = [buf("bank%d" % i) for i in range(8)]
        bank_rr = [0]

        def bank():
            k = bank_rr[0] % 8
            bank_rr[0] += 1
            return Reg(psum[k // 2][:, (k % 2) * 512:(k % 2) * 512 + 512], [bank_bufs[k]])

        def bank2():
            if bank_rr[0] % 2:
                bank_rr[0] += 1
            k = bank_rr[0] % 8
            bank_rr[0] += 2
            return Reg(psum[k // 2][:, :], [bank_bufs[k], bank_bufs[k + 1]])

        TAb = [buf("ta%d" % i) for i in range(40)]

        def ta(off_f32, ncols, dt=F32):
            nf = ncols if dt == F32 else (ncols + 1) // 2
            ap = TA[:, off_f32:off_f32 + nf]
            if dt != F32:
                ap = ap.bitcast(dt)
            b0, b1 = off_f32 // 256, (off_f32 + nf - 1) // 256
            return Reg(ap, TAb[b0:b1 + 1])

        WAb = [buf("wa%d" % i) for i in range(5)]

        def wa(off, ncols):
            b0, b1 = off // 4096, (off + ncols - 1) // 4096
            return Reg(WA[:, off:off + ncols], WAb[b0:b1 + 1])

        XAb = [buf("xa%d" % i) for i in range(16 * S // 1024)]

        def xa(off, ncols):
            b0, b1 = off // 1024, (off + ncols - 1) // 1024
            return Reg(XA[:, off:off + ncols], XAb[b0:b1 + 1])

        XAf = XA[:, :].bitcast(F32)

        def xacc(j):
            return Reg(XAf[:, j * 1024:(j + 1) * 1024], XAb[2 * j:2 * j + 2])

        hTb = [buf("hT%d" % j) for j in range(NT)]
        hT3 = hT[:, :].rearrange("p (k s) -> p k s", s=S)

        ident_f = cst[:, 0:128]
        mask_bd = cst[:, 256:384]
        causal = cst[:, 384:512]
        ident_b = cstb[:, 0:128]
        ones_b = cstb[:, 128:256]
        bsmall = buf("small")
        badaT = buf("adaT")
        bscl = buf("scl")
        bvec = buf("vecfm")
        bgt = [buf("gt1bc"), buf("gt2bc")]
        bconst = buf("consts2")
        blg = buf("lg")
        bcomb = buf("comb")
        brt = buf("rtmp")

        def r32(n):
            return 32 if n <= 32 else (64 if n <= 64 else 128)

        def pmode(lhsT, kind="m"):
            return (kind, r32(lhsT.shape[0]), r32(lhsT.shape[-1]), str(lhsT.dtype))

        def mm(out, lhsT, rhs, start, stop, r, w):
            P.c("pe", lambda h: h.matmul(out, lhsT, rhs, start=start, stop=stop), r=r, w=w, mode=pmode(lhsT))

        def act(out, in_, func, r, w, **kw):
            P.c("act", lambda h: h.activation(out, in_, func, **kw), r=r, w=w)

        def dve(fn, r, w):
            P.c("dve", fn, r=r, w=w)

        def pool(fn, r, w):
            P.c("pool", fn, r=r, w=w)

        def ld(eng, out, in_, dst, w):
            P.dma(eng, lambda h: h.dma_start(out=out, in_=in_), dst, w=w)

        def tt(out, in0, in1, op, r, w):
            dve(lambda h: h.tensor_tensor(out=out, in0=in0, in1=in1, op=op), r, w)

        def ts(out, in0, s1, s2, op0, op1, r, w):
            if s2 is None:
                dve(lambda h: h.tensor_scalar(out=out, in0=in0, scalar1=s1, scalar2=None, op0=op0), r, w)
            else:
                dve(lambda h: h.tensor_scalar(out=out, in0=in0, scalar1=s1, scalar2=s2, op0=op0, op1=op1), r, w)

        def stt(out, in0, scalar, in1, op0, op1, r, w):
            dve(lambda h: h.scalar_tensor_tensor(out=out, in0=in0, scalar=scalar, in1=in1, op0=op0, op1=op1), r, w)

        def bc_last(ap2, n):
            return ap2.unsqueeze(2).to_broadcast([128, ap2.shape[1], n])

        def bc_mid(ap2, a):
            return ap2.unsqueeze(1).to_broadcast([128, a, ap2.shape[1]])

        ld("sp", cst[:, :], cst_d[:, 0:768], buf("cst"), [B["cst"]])
        ld("pool", cstb[:, 0:256], cst_d[:, 0:256], buf("cstb"), [B["cstb"]])
        ld("sp", vecfm[:, :], vecfm_d.rearrange("p a b -> p (a b)"), bvec, [bvec])
        ld("sp", brbc[:, :], brbc_d, buf("brbc"), [B["brbc"]])
        ld("pool", wrb[:, :].rearrange("p (k n) -> p k n", n=20), wr_d.rearrange("(k p) n -> p k n", p=128), buf("wrb"), [B["wrb"]])
        VF = vecfm[:, :].rearrange("p (a b) -> p a b", b=8)
        V_GPRE, V_LNG, V_LB0, V_LB1, V_GH, V_GPF, V_GPM, V_GPFF = range(8)

        EPS6 = small[:, 0:1]
        EPS5 = small[:, 1:2]
        dve(lambda h: h.memset(small[:, 0:1], 1e-6), [], [bsmall])
        dve(lambda h: h.memset(small[:, 1:2], 1e-5), [], [bsmall])
        EPS6X4 = small[:, 4:5]
        dve(lambda h: h.memset(small[:, 4:5], 4e-6), [], [bsmall])
        NEG1 = small[:, 2:3]
        NEGH = small[:, 3:4]
        dve(lambda h: h.memset(small[:, 2:3], -1.0), [], [bsmall])
        dve(lambda h: h.memset(small[:, 3:4], -0.5), [], [bsmall])
        FCA = small[:, 8:16]
        FCB = small[:, 16:24]
        GQ = small[:, 24:32]
        tt(small[:, 32:40], VF[:, V_LB0, :], VF[:, V_LB1, :], ALU.subtract, [bvec], [bsmall])
        act(small[:, 32:40], small[:, 32:40], AF.Tanh, [bsmall], [bsmall], scale=0.5)
        ts(FCA, small[:, 32:40], -0.25, 0.25, ALU.mult, ALU.add, [bsmall], [bsmall])
        ts(FCB, small[:, 32:40], 0.25, 0.75, ALU.mult, ALU.add, [bsmall], [bsmall])
        ts(GQ, VF[:, V_GH, :], 0.25, None, ALU.mult, None, [bvec], [bsmall])

        wsp = ta(0, 1024)
        ld("sp", wsp.ap, wspT_d.rearrange("p a b -> p (a b)"), wsp.bufs[0], wsp.bufs)
        tt(wcT[:, :].rearrange("p (g t) -> p g t", t=128), wsp.ap.rearrange("p (g t) -> p g t", t=128), bc_mid(causal, 8),
           ALU.mult, wsp.bufs + [B["cst"]], [bconst])
        l2 = ta(1024, 1024)
        r2t = ta(2048, 1024)
        ld("sp", l2.ap[0:2, :], lnb2_d, l2.bufs[0], l2.bufs)
        ld("sp", r2t.ap[1:2, :], bsp_d, r2t.bufs[0], r2t.bufs)
        pb2 = bank2()
        for half in range(2):
            mm(pb2.ap[0:1, half * 512:(half + 1) * 512], ones_b[:, 0:1], wcT[:, half * 512:(half + 1) * 512], True, True,
               [B["cstb"], bconst], pb2.bufs)
        dve(lambda h, pb2=pb2: h.tensor_copy(out=r2t.ap[0:1, :], in_=pb2.ap[0:1, :]), pb2.bufs, r2t.bufs)
        pb2 = bank2()
        for g in range(8):
            mm(pb2.ap[:, g * 128:(g + 1) * 128], l2.ap[0:2, g * 128:(g + 1) * 128], r2t.ap[0:2, g * 128:(g + 1) * 128], True, True,
               l2.bufs + r2t.bufs, pb2.bufs)
        dve(lambda h, pb2=pb2: h.tensor_copy(out=Rsb[:, :], in_=pb2.ap), pb2.bufs, [bconst])

        cTt = ta(3072, 16)
        ld("sp", cTt.ap, cT_d.rearrange("p a b -> p (a b)"), buf("cT"), cTt.bufs + [B["cT"]])
        thc = ta(3328, 16)
        act(thc.ap, cTt.ap, AF.Tanh, cTt.bufs + [B["cT"]], thc.bufs, scale=0.5)
        sc_b = ta(3584, 16, BF16)
        stt(thc.ap, thc.ap, 1.0, cTt.ap, ALU.add, ALU.mult, thc.bufs + cTt.bufs, thc.bufs)
        ts(sc_b.ap, thc.ap, 0.5, None, ALU.mult, None, thc.bufs, sc_b.bufs)
        scb3 = sc_b.ap.rearrange("p (k b) -> p k b", b=2)
        adar = ta(4096, 6144)
        adasb = adar.ap
        bada = adar.bufs
        ld("sp", adasb[0:2, :], bada_d, bada[0], bada)
        wada_v = wada_d.rearrange("(k p) n -> p k n", p=128)
        for nb in range(12):
            slot = wa((nb % 2) * 4096, 4096)
            w3v = slot.ap.rearrange("p (k n) -> p k n", n=512)
            ld("pool", w3v, wada_v[:, :, nb * 512:(nb + 1) * 512], slot.bufs[0], slot.bufs)
            pb = bank()
            for kc in range(8):
                mm(pb.ap[0:2, :], scb3[:, kc, :], w3v[:, kc, :], kc == 0, kc == 7, sc_b.bufs + slot.bufs, pb.bufs)
            tt(adasb[0:2, nb * 512:(nb + 1) * 512], pb.ap[0:2, :], adasb[0:2, nb * 512:(nb + 1) * 512], ALU.add,
               pb.bufs + bada, bada)
        for sl in (1, 4):
            ts(adasb[0:2, sl * D:(sl + 1) * D], adasb[0:2, sl * D:(sl + 1) * D], 1.0, None, ALU.add, None, bada, bada)
        pb = bank()
        for sl in range(6):
            for kc in range(8):
                col = (sl * 8 + kc) * 2
                mm(pb.ap[:, col:col + 2], adasb[0:2, sl * D + kc * 128: sl * D + (kc + 1) * 128], ident_f[0:2, 0:2],
                   True, True, bada + [B["cst"]], pb.bufs)
        dve(lambda h, pb=pb: h.tensor_copy(out=adaT[:, 0:96], in_=pb.ap[:, 0:96]), pb.bufs, [badaT])
        aT4 = adaT[:, 0:96].rearrange("p (s k b) -> p s k b", s=6, k=8)
        scl4 = scl[:, :].rearrange("p (s k b) -> p s k b", s=6, k=8)
        for b in range(2):
            tt(scl4[:, 0, :, b], aT4[:, 1, :, b], VF[:, V_GPRE, :], ALU.mult, [badaT, bvec], [bscl])
            tt(scl4[:, 2, :, b], aT4[:, 4, :, b], VF[:, V_GPF, :], ALU.mult, [badaT, bvec], [bscl])
            tt(scl4[:, 4, :, b], aT4[:, 2, :, b], VF[:, V_GPM, :], ALU.mult, [badaT, bvec], [bscl])
            tt(scl4[:, 5, :, b], aT4[:, 5, :, b], VF[:, V_GPFF, :], ALU.mult, [badaT, bvec], [bscl])
            dve(lambda h, b=b: h.tensor_copy(out=scl4[:, 1, :, b], in_=aT4[:, 0, :, b]), [badaT], [bscl])
            dve(lambda h, b=b: h.tensor_copy(out=scl4[:, 3, :, b], in_=aT4[:, 3, :, b]), [badaT], [bscl])

        def make_gtbc(which, b):
            dg = ta(9216, 1024)
            for kc in range(8):
                ts(dg.ap[:, kc * 128:(kc + 1) * 128], ident_f, scl4[:, 4 + which, kc, b:b + 1], None, ALU.mult, None,
                   [B["cst"], bscl], dg.bufs)
            pbg = bank2()
            for kc in range(8):
                mm(pbg.ap[:, kc * 128:(kc + 1) * 128], cst[:, 128:256], dg.ap[:, kc * 128:(kc + 1) * 128], True, True,
                   dg.bufs + [B["cst"]], pbg.bufs)
            dve(lambda h: h.tensor_copy(out=gtbc[:, which * D:(which + 1) * D], in_=pbg.ap), pbg.bufs, [bgt[which]])

        def phase1(s):
                ssq = small[:, 64:64 + NT]
                rstd = small[:, 80:80 + NT]
                xt = [ta(0, 1024), ta(1024, 1024)]
                junk = ta(2048, 1024, BF16)
                for j in range(NT):
                    xs = xt[j % 2]
                    ld("sp", xs.ap, x_d[s, j * 128:(j + 1) * 128, :], xs.bufs[0], xs.bufs)
                    act(junk.ap, xs.ap, AF.Square, xs.bufs, junk.bufs + [bsmall], accum_out=ssq[:, j:j + 1])
                act(rstd, ssq, AF.Sqrt, [bsmall], [bsmall], bias=EPS6, scale=1.0 / D)
                dve(lambda h: h.reciprocal(out=rstd, in_=rstd), [bsmall], [bsmall])
                xn = [ta(2560, 1024, BF16), ta(3072, 1024, BF16)]
                for j in range(NT):
                    xs = xt[j % 2]
                    ld("sp", xs.ap, x_d[s, j * 128:(j + 1) * 128, :], xs.bufs[0], xs.bufs)
                    xb = xn[j % 2]
                    act(xb.ap, xs.ap, AF.Copy, xs.bufs + [bsmall], xb.bufs, scale=rstd[:, j:j + 1])
                    pb = bank()
                    pbb = pb.ap.bitcast(BF16)
                    for kc in range(8):
                        P.c("pe", lambda h, pbb=pbb, xb=xb, kc=kc: h.transpose(pbb[:, kc * 128:(kc + 1) * 128],
                                                                                xb.ap[:, kc * 128:(kc + 1) * 128], ident_b),
                            r=xb.bufs + [B["cstb"]], w=pb.bufs, mode=("t", 128, 128))
                    for kc in range(8):
                        ts(hT3[:, kc, j * 128:(j + 1) * 128], pbb[:, kc * 128:(kc + 1) * 128],
                           scl4[:, 0, kc, s:s + 1], scl4[:, 1, kc, s:s + 1], ALU.mult, ALU.add, pb.bufs + [bscl], [hTb[j]])

        def prefetch_mixer_weights():
            wvs_ = wa(0, 8192)
            ld("pool", wvs_.ap.rearrange("p (k n) -> p k n", n=1024), wv_d.rearrange("(k p) n -> p k n", p=128), wvs_.bufs[0], wvs_.bufs)
            w0 = wa(8192, 7168)
            ld("pool", w0.ap.rearrange("p (k n) -> p k n", n=896), wfm_d[0].rearrange("(k p) n -> p k n", p=128), w0.bufs[0], w0.bufs)

        for s in range(2):
            if s == 0:
                prefetch_mixer_weights()
                phase1(0)
            if upto <= 1:
                break

            wvs = wa(0, 8192)
            wv3 = wvs.ap.rearrange("p (k n) -> p k n", n=1024)
            s1 = small[:, 96:96 + NT]
            s2 = small[:, 112:112 + NT]
            rsv = small[:, 128:128 + NT]
            nbv = small[:, 144:144 + NT]
            junk2 = ta(0, 1024, BF16)
            vn = [xa(j * 1024, 1024) for j in range(NT)]
            for j in range(NT):
                pb2 = bank2()
                for half in range(2):
                    for kc in range(8):
                        mm(pb2.ap[:, half * 512:(half + 1) * 512], hT3[:, kc, j * 128:(j + 1) * 128],
                           wv3[:, kc, half * 512:(half + 1) * 512], kc == 0, kc == 7, [hTb[j]] + wvs.bufs, pb2.bufs)
                act(vn[j].ap, pb2.ap, AF.Gelu_apprx_tanh, pb2.bufs, vn[j].bufs + [bsmall], accum_out=s1[:, j:j + 1])
                act(junk2.ap, vn[j].ap, AF.Square, vn[j].bufs, junk2.bufs + [bsmall], accum_out=s2[:, j:j + 1])
            ts(s1, s1, 1.0 / D, None, ALU.mult, None, [bsmall], [bsmall])
            ts(s2, s2, 1.0 / D, None, ALU.mult, None, [bsmall], [bsmall])
            tt(rsv, s1, s1, ALU.mult, [bsmall], [bsmall])
            tt(s2, s2, rsv, ALU.subtract, [bsmall], [bsmall])
            act(rsv, s2, AF.Sqrt, [bsmall], [bsmall], bias=EPS5, scale=1.0)
            dve(lambda h: h.reciprocal(out=rsv, in_=rsv), [bsmall], [bsmall])
            stt(nbv, s1, -1.0, rsv, ALU.mult, ALU.mult, [bsmall], [bsmall])
            for j in range(NT):
                if j % 2 == 0:
                    act(vn[j].ap, vn[j].ap, AF.Identity, vn[j].bufs + [bsmall], vn[j].bufs, bias=nbv[:, j:j + 1], scale=rsv[:, j:j + 1])
                else:
                    ts(vn[j].ap, vn[j].ap, rsv[:, j:j + 1], nbv[:, j:j + 1], ALU.mult, ALU.add, vn[j].bufs + [bsmall], vn[j].bufs)

            if STOP == "a":
                break
            YT0 = NT * 1024
            F_ = ta(0, 512); Pc = ta(512, 512); RP = ta(1024, 512); D1 = ta(1536, 512); THQ = ta(2048, 512)
            IT = ta(2560, 512, BF16); KH = ta(2816, 512, BF16)
            KP = [ta(3072 + 256 * i, 512, BF16) for i in range(2)]
            QH = [ta(3584 + 256 * i, 512, BF16) for i in range(2)]
            ITOK = [ta(4096 + 256 * i, 512, BF16) for i in range(2)]
            KHM = [[ta(4608, 512, BF16), ta(4864, 512, BF16)], [ta(8320, 512, BF16), ta(8576, 512, BF16)]]
            PLAST = [small[:, 160 + 8 * i:168 + 8 * i] for i in range(2)]
            bpl = [buf("plast0"), buf("plast1")]
            A_SB = ta(5120, 512, BF16); Sst = ta(5376, 128); S_BF = ta(5504, 1024, BF16)
            OSQ = ta(6016, 512, BF16); RT = ta(6272, 512); ON = ta(6784, 512); THOG = ta(7296, 512); THGB = ta(7808, 512)
            GU = ta(9856, 512, BF16); THGA = ta(8832, 512); ZV = ta(9344, 512)
            dve(lambda h: h.memset(D1.ap, 0.0), [], D1.bufs)
            for sl_ in range(2):
                dve(lambda h, sl_=sl_: h.memset(KHM[sl_][0].ap[64:128, :], 0.0), [], KHM[sl_][0].bufs)
                dve(lambda h, sl_=sl_: h.memset(KHM[sl_][1].ap[0:64, :], 0.0), [], KHM[sl_][1].bufs)
            OFF = dict(u=0, ga=128, q=256, f=384, i=512, og=640, gb=768)

            def wslot(h):
                return wa(((h + 1) % 2) * 8192, 7168)

            def proj(h, tb, name, wsl):
                w3_ = wsl.ap.rearrange("p (k n) -> p k n", n=896)
                pb = bank()
                o = OFF[name]
                for kc in range(8):
                    mm(pb.ap, w3_[:, kc, o:o + 128], hT3[:, kc, tb * 512:(tb + 1) * 512], kc == 0, kc == 7,
                       wsl.bufs + hTb[tb * 4:tb * 4 + 4], pb.bufs)
                return pb

            def stage1(h, tb, sl):
                wsl = wslot(h)
                pbf = proj(h, tb, "f", wsl)
                act(F_.ap, pbf.ap, AF.Tanh, pbf.bufs, F_.bufs, scale=0.5)
                pbq = proj(h, tb, "q", wsl)
                act(THQ.ap, pbq.ap, AF.Tanh, pbq.bufs, THQ.bufs, scale=0.5)
                pbi = proj(h, tb, "i", wsl)
                act(IT.ap, pbi.ap, AF.Copy, pbi.bufs, IT.bufs)
                act(F_.ap, F_.ap, AF.Identity, F_.bufs + [bsmall], F_.bufs, scale=FCA[:, h:h + 1], bias=FCB[:, h:h + 1])
                f3 = F_.ap.rearrange("p (c j) -> p c j", j=64)
                d3 = D1.ap.rearrange("p (c j) -> p c j", j=64)
                p3 = Pc.ap.rearrange("p (c j) -> p c j", j=64)
                act(d3[:, :, 0:1], f3[:, :, 0:1], AF.Copy, F_.bufs, D1.bufs)
                dve(lambda hh: hh.tensor_tensor_scan(out=Pc.ap, data0=F_.ap, data1=D1.ap, initial=1.0, op0=ALU.mult, op1=ALU.max),
                    F_.bufs + D1.bufs, Pc.bufs)
                act(PLAST[sl], p3[:, :, 63], AF.Copy, Pc.bufs, [bpl[sl]])
                stt(THQ.ap, THQ.ap, 1.0, pbq.ap, ALU.add, ALU.mult, THQ.bufs + pbq.bufs, THQ.bufs)
                pool(lambda hh: hh.tensor_scalar(out=F_.ap, in0=F_.ap, scalar1=-1.0, scalar2=1.0, op0=ALU.mult, op1=ALU.add),
                     F_.bufs, F_.bufs)
                dve(lambda hh: hh.reciprocal(out=RP.ap, in_=Pc.ap), Pc.bufs, RP.bufs)
                pool(lambda hh: hh.tensor_tensor(out=KP[sl].ap, in0=F_.ap, in1=RP.ap, op=ALU.mult),
                     RP.bufs + F_.bufs, KP[sl].bufs)
                pool(lambda hh: hh.tensor_tensor(out=KH.ap.rearrange("p (c j) -> p c j", j=64),
                                                 in0=KP[sl].ap.rearrange("p (c j) -> p c j", j=64),
                                                 in1=bc_last(PLAST[sl], 64), op=ALU.mult),
                     KP[sl].bufs + [bpl[sl]], KH.bufs)
                pool(lambda hh: hh.tensor_tensor(out=QH[sl].ap, in0=THQ.ap, in1=Pc.ap, op=ALU.mult),
                     THQ.bufs + Pc.bufs, QH[sl].bufs)
            def stage1b(h, tb, sl):
                pbt = bank()
                pbtb = pbt.ap.bitcast(BF16)
                for jj in range(4):
                    P.c("pe", lambda hh, jj=jj: hh.transpose(pbtb[:, jj * 128:(jj + 1) * 128], IT.ap[:, jj * 128:(jj + 1) * 128], ident_b),
                        r=IT.bufs + [B["cstb"]], w=pbt.bufs, mode=("t", 128, 128))
                for jj in range(4):
                    P.c("pe", lambda hh, jj=jj: hh.transpose(pbtb[:, 512 + jj * 128:512 + (jj + 1) * 128],
                                                             KH.ap[:, jj * 128:(jj + 1) * 128], ident_b),
                        r=KH.bufs + [B["cstb"]], w=pbt.bufs, mode=("t", 128, 128))
                act(ITOK[sl].ap, pbtb[:, 0:512], AF.Copy, pbt.bufs, ITOK[sl].bufs)
                act(KHM[sl][0].ap[0:64, :], pbtb[0:64, 512:1024], AF.Copy, pbt.bufs, KHM[sl][0].bufs)
                act(KHM[sl][1].ap[64:128, :], pbtb[64:128, 512:1024], AF.Copy, pbt.bufs, KHM[sl][1].bufs)

            def stage2(h, tb, sl):
                if STOP == "b":
                    return
                wsl = wslot(h)
                pbs = bank()
                for jj in range(4):
                    c0 = jj * 128
                    mm(pbs.ap[:, c0:c0 + 128], KP[sl].ap[:, c0:c0 + 128], QH[sl].ap[:, c0:c0 + 128], True, True,
                       KP[sl].bufs + QH[sl].bufs, pbs.bufs)
                tt(A_SB.ap.rearrange("p (a t) -> p a t", t=128), pbs.ap.rearrange("p (a t) -> p a t", t=128), bc_mid(mask_bd, 4),
                   ALU.mult, pbs.bufs + [B["cst"]], A_SB.bufs)
                if STOP == "c":
                    return
                pbus = [bank(), bank()]
                def ureg(cc):
                    return pbus[cc // 4], pbus[cc // 4].ap[:, (cc % 4) * 128:(cc % 4 + 1) * 128]
                for cc in range(8):
                    jj, hh_ = cc // 2, cc % 2
                    mm(ureg(cc)[1], KHM[sl][hh_].ap[:, jj * 128:(jj + 1) * 128],
                       ITOK[sl].ap[:, jj * 128:(jj + 1) * 128], True, True,
                       KHM[sl][hh_].bufs + ITOK[sl].bufs, ureg(cc)[0].bufs)
                SS = [Sst, Reg(QFt[:, 0:128], [buf("QF")])]
                if tb == 0:
                    dve(lambda hh: hh.memset(SS[0].ap, 0.0), [], SS[0].bufs)
                for cc in range(8):
                    cur, nx = SS[cc % 2], SS[(cc + 1) % 2]
                    act(S_BF.ap[:, cc * 128:(cc + 1) * 128], cur.ap, AF.Copy, cur.bufs, [buf("sbf%d" % cc)])
                    stt(nx.ap, cur.ap, PLAST[sl][:, cc:cc + 1], ureg(cc)[1], ALU.mult, ALU.add,
                        cur.bufs + [bpl[sl]] + ureg(cc)[0].bufs, nx.bufs)
                pbog = proj(h, tb, "og", wsl)
                act(THOG.ap, pbog.ap, AF.Tanh, pbog.bufs, THOG.bufs, scale=0.5)
                pbgb = proj(h, tb, "gb", wsl)
                act(THGB.ap, pbgb.ap, AF.Tanh, pbgb.bufs, THGB.bufs, scale=0.5)
                stt(THOG.ap, THOG.ap, 1.0, pbog.ap, ALU.add, ALU.mult, THOG.bufs + pbog.bufs, THOG.bufs)
                stt(THGB.ap, THGB.ap, 1.0, THOG.ap, ALU.add, ALU.mult, THGB.bufs + THOG.bufs, THGB.bufs)
                pbuu = proj(h, tb, "u", wsl)
                act(GU.ap, pbuu.ap, AF.Gelu_apprx_tanh, pbuu.bufs, GU.bufs)
                pbga = proj(h, tb, "ga", wsl)
                act(THGA.ap, pbga.ap, AF.Tanh, pbga.bufs, THGA.bufs, scale=0.5)
                pbz = bank()
                for jj in range(4):
                    j = tb * 4 + jj
                    mm(pbz.ap[:, jj * 128:(jj + 1) * 128], vn[j].ap[:, h * 128:(h + 1) * 128], wcT[:, h * 128:(h + 1) * 128],
                       True, True, vn[j].bufs + [bconst], pbz.bufs)
                stt(ZV.ap.rearrange("p (a t) -> p a t", t=128), pbz.ap.rearrange("p (a t) -> p a t", t=128), VF[:, V_LNG, h:h + 1],
                    bc_mid(Rsb[:, h * 128:(h + 1) * 128], 4), ALU.mult, ALU.add, pbz.bufs + [bvec, bconst], ZV.bufs)
                stt(THGA.ap, THGA.ap, 1.0, GU.ap, ALU.add, ALU.mult, THGA.bufs + GU.bufs, THGA.bufs)
                stt(ZV.ap, THGA.ap, 0.5, ZV.ap, ALU.mult, ALU.mult, THGA.bufs + ZV.bufs, ZV.bufs)
            def stage2b(h, tb, sl):
                pbo = bank()
                for jj in range(4):
                    c0 = jj * 128
                    mm(pbo.ap[:, c0:c0 + 128], ITOK[sl].ap[:, c0:c0 + 128], A_SB.ap[:, c0:c0 + 128], True, False,
                       ITOK[sl].bufs + A_SB.bufs, pbo.bufs)
                    for cc in (2 * jj, 2 * jj + 1):
                        c1 = cc * 64
                        P.c("pe", lambda hh, c1=c1, cc=cc: hh.matmul(pbo.ap[:, c1:c1 + 64], S_BF.ap[:, cc * 128:(cc + 1) * 128],
                                                                      QH[sl].ap[:, c1:c1 + 64], start=False, stop=(cc % 2 == 1)),
                            r=[buf("sbf%d" % cc)] + QH[sl].bufs, w=pbo.bufs, mode=pmode(S_BF.ap[:, 0:128]))
                act(OSQ.ap, pbo.ap, AF.Square, pbo.bufs, OSQ.bufs)
                pbn = bank()
                mm(pbn.ap, ones_b, OSQ.ap, True, True, OSQ.bufs + [B["cstb"]], pbn.bufs)
                act(RT.ap, pbn.ap, AF.Ln, pbn.bufs + [bsmall], RT.bufs, bias=EPS6X4, scale=1.0 / 128)
                act(RT.ap, RT.ap, AF.Exp, RT.bufs, RT.bufs, scale=-0.5)
                pool(lambda hh: hh.tensor_tensor(out=THGB.ap, in0=THGB.ap, in1=RT.ap, op=ALU.mult), THGB.bufs + RT.bufs, THGB.bufs)
                stt(ON.ap, pbo.ap, GQ[:, h:h + 1], THGB.ap, ALU.mult, ALU.mult, pbo.bufs + THGB.bufs + [bsmall], ON.bufs)
                yreg = xa(YT0 + h * S + tb * 512, 512)
                pool(lambda hh: hh.tensor_tensor(out=yreg.ap, in0=ZV.ap, in1=ON.ap, op=ALU.add), ZV.bufs + ON.bufs, yreg.bufs)

            def load_wfm(h):
                wsl = wslot(h)
                ld("pool", wsl.ap.rearrange("p (k n) -> p k n", n=896), wfm_d[h].rearrange("(k p) n -> p k n", p=128),
                   wsl.bufs[0], wsl.bufs)

            wos = wa(8192, 8192)
            wo3 = wos.ap.rearrange("p (k n) -> p k n", n=1024)
            its = [(h, tb) for h in range(8) for tb in range(NB)]
            load_wfm(1)
            stage1(its[0][0], its[0][1], 0)
            stage1b(its[0][0], its[0][1], 0)
            for k in range(len(its)):
                nxt = k + 1 < len(its)
                if nxt:
                    stage1(its[k + 1][0], its[k + 1][1], (k + 1) % 2)
                stage2(its[k][0], its[k][1], k % 2)
                if nxt:
                    stage1b(its[k + 1][0], its[k + 1][1], (k + 1) % 2)
                stage2b(its[k][0], its[k][1], k % 2)
                if its[k][1] == NB - 1:
                    if its[k][0] + 2 < 8:
                        load_wfm(its[k][0] + 2)
                    elif its[k][0] == 6:
                        ld("pool", wo3, wout_d.rearrange("(k p) n -> p k n", p=128), wos.bufs[0], wos.bufs)
            if upto <= 2:
                break

            make_gtbc(0, s)
            XR = [ta(0, 1024), ta(1024, 1024)]
            X1 = [ta(2048, 1024), ta(3072, 1024)]
            junk3 = ta(4096, 1024, BF16)
            XN = [ta(4608, 1024, BF16), ta(5120, 1024, BF16)]
            sq2 = small[:, 176:176 + 2 * NT]
            lg3 = lg[:, :].rearrange("p (t n) -> p t n", n=20)
            bx1 = buf("x1s%d" % s)
            yT3 = XA[:, YT0:YT0 + 8 * S].rearrange("p (k s) -> p k s", s=S)
            def p3a(j):
                pb2 = bank2()
                for half in range(2):
                    for kc in range(8):
                        yr = xa(YT0 + kc * S + j * 128, 128)
                        mm(pb2.ap[:, half * 512:(half + 1) * 512], yT3[:, kc, j * 128:(j + 1) * 128],
                           wo3[:, kc, half * 512:(half + 1) * 512], kc == 0, kc == 7, yr.bufs + wos.bufs, pb2.bufs)
                xr = XR[j % 2]
                ld("sp", xr.ap, x_d[s, j * 128:(j + 1) * 128, :], xr.bufs[0], xr.bufs)
                return pb2

            def p3b(j, pb2):
                xr = XR[j % 2]
                r2c = sq2[:, 2 * j:2 * j + 1]
                r3c = sq2[:, 2 * j + 1:2 * j + 2]
                act(junk3.ap, pb2.ap, AF.Square, pb2.bufs, junk3.bufs + [bsmall], accum_out=r2c)
                act(r2c, r2c, AF.Ln, [bsmall], [bsmall], bias=EPS6, scale=1.0 / D)
                act(r2c, r2c, AF.Exp, [bsmall], [bsmall], scale=-0.5)
                x1 = X1[j % 2]
                stt(x1.ap, pb2.ap, r2c, gtbc[:, 0:D], ALU.mult, ALU.mult, pb2.bufs + [bsmall, bgt[0]], x1.bufs)
                tt(x1.ap, x1.ap, xr.ap, ALU.add, x1.bufs + xr.bufs, x1.bufs)
                P.dma("sp", lambda h, x1=x1, j=j, s=s: h.dma_start(out=x1s_d[s, j * 128:(j + 1) * 128, :], in_=x1.ap),
                      bx1, r=x1.bufs, w=[bx1])
                act(junk3.ap, x1.ap, AF.Square, x1.bufs, junk3.bufs + [bsmall], accum_out=r3c)
                act(r3c, r3c, AF.Ln, [bsmall], [bsmall], bias=EPS6, scale=1.0 / D)
                act(r3c, r3c, AF.Exp, [bsmall], [bsmall], scale=-0.5)
                xb = XN[j % 2]
                act(xb.ap, x1.ap, AF.Copy, x1.bufs + [bsmall], xb.bufs, scale=r3c)

            def p3c(j):
                xb = XN[j % 2]
                pb = bank()
                pbb = pb.ap.bitcast(BF16)
                for kc in range(8):
                    P.c("pe", lambda h, pbb=pbb, xb=xb, kc=kc: h.transpose(pbb[:, kc * 128:(kc + 1) * 128],
                                                                            xb.ap[:, kc * 128:(kc + 1) * 128], ident_b),
                        r=xb.bufs + [B["cstb"]], w=pb.bufs, mode=("t", 128, 128))
                for kc in range(8):
                    ts(hT3[:, kc, j * 128:(j + 1) * 128], pbb[:, kc * 128:(kc + 1) * 128],
                       scl4[:, 2, kc, s:s + 1], scl4[:, 3, kc, s:s + 1], ALU.mult, ALU.add, pb.bufs + [bscl], [hTb[j]])
                pbr = bank()
                for kc in range(8):
                    mm(pbr.ap[:, 0:20], hT3[:, kc, j * 128:(j + 1) * 128], wrb[:, kc * 20:(kc + 1) * 20], kc == 0, kc == 7,
                       [hTb[j], B["wrb"]], pbr.bufs)
                tt(lg3[:, j, :], pbr.ap[:, 0:20], brbc[:, :], ALU.add, pbr.bufs + [B["brbc"]], [blg])

            pcur = p3a(0)
            for j in range(NT):
                pnext = p3a(j + 1) if j + 1 < NT else None
                p3b(j, pcur)
                p3c(j)
                pcur = pnext
            if upto <= 3:
                break

            RTm = rtmp[:, :]
            def rt(off, n):
                return RTm[:, off * NT:(off + n) * NT]
            G = lg3[:, :, 0:4]
            E4 = lg3[:, :, 4:20].rearrange("p t (g e) -> p t g e", e=4)
            gmax = rt(0, 1); ohg = rt(1, 4).rearrange("p (t g) -> p t g", g=4); dG = rt(5, 4).rearrange("p (t g) -> p t g", g=4)
            sumg = rt(9, 1); EM = rt(10, 16).rearrange("p (t g e) -> p t g e", g=4, e=4)
            El = rt(26, 4).rearrange("p (t e) -> p t e", e=4); m1 = rt(30, 1); oh1 = rt(31, 4).rearrange("p (t e) -> p t e", e=4)
            El2 = rt(35, 4).rearrange("p (t e) -> p t e", e=4); m2 = rt(39, 1); oh2 = rt(40, 4).rearrange("p (t e) -> p t e", e=4)
            rr = rt(44, 1); wa_ = rt(45, 1); wb_ = rt(46, 1); ce = rt(47, 4).rearrange("p (t e) -> p t e", e=4)
            ce2 = rt(51, 4).rearrange("p (t e) -> p t e", e=4)
            comb4 = comb[:, :].rearrange("p (t g e) -> p t g e", g=4, e=4)
            RB = [brt]
            dve(lambda h: h.tensor_reduce(out=gmax, in_=G, axis=AX.X, op=ALU.max), [blg], RB)
            tt(ohg, G, bc_last(gmax, 4), ALU.is_equal, [blg] + RB, RB)
            tt(dG, G, bc_last(gmax, 4), ALU.subtract, [blg] + RB, RB)
            act(dG, dG, AF.Exp, RB, RB)
            dve(lambda h: h.tensor_reduce(out=sumg, in_=dG, axis=AX.X, op=ALU.add), RB, RB)
            dve(lambda h: h.reciprocal(out=sumg, in_=sumg), RB, RB)
            tt(EM, E4, ohg.unsqueeze(3).to_broadcast([128, NT, 4, 4]), ALU.mult, [blg] + RB, RB)
            dve(lambda h: h.tensor_reduce(out=El, in_=EM.rearrange("p t g e -> p t e g"), axis=AX.X, op=ALU.add), RB, RB)
            dve(lambda h: h.tensor_reduce(out=m1, in_=El, axis=AX.X, op=ALU.max), RB, RB)
            tt(oh1, El, bc_last(m1, 4), ALU.is_equal, RB, RB)
            stt(El2, oh1, -1e30, El, ALU.mult, ALU.add, RB, RB)
            dve(lambda h: h.tensor_reduce(out=m2, in_=El2, axis=AX.X, op=ALU.max), RB, RB)
            tt(oh2, El2, bc_last(m2, 4), ALU.is_equal, RB, RB)
            tt(rr, m2, m1, ALU.subtract, RB, RB)
            act(rr, rr, AF.Exp, RB, RB)
            ts(wa_, rr, 1.0, None, ALU.add, None, RB, RB)
            dve(lambda h: h.reciprocal(out=wa_, in_=wa_), RB, RB)
            tt(wa_, wa_, sumg, ALU.mult, RB, RB)
            tt(wb_, wa_, rr, ALU.mult, RB, RB)
            tt(ce, oh1, bc_last(wa_, 4), ALU.mult, RB, RB)
            tt(ce2, oh2, bc_last(wb_, 4), ALU.mult, RB, RB)
            tt(ce, ce, ce2, ALU.add, RB, RB)
            dve(lambda h: h.tensor_copy(out=comb4, in_=ce.unsqueeze(2).to_broadcast([128, NT, 4, 4])), RB, [bcomb])
            tt(comb4, comb4, ohg.unsqueeze(3).to_broadcast([128, NT, 4, 4]), ALU.mult, RB + [bcomb], [bcomb])
            comb3 = comb[:, :].rearrange("p (t e) -> p t e", e=16)

            AT = [ta(0, 2048, BF16), ta(1024, 2048, BF16)]
            S1 = [ta(2048, 512, BF16), ta(2304, 512, BF16)]
            def moe_w(e):
                sl = e % 2
                w1s = wa(sl * 8192, 4096); w3s = wa(sl * 8192 + 4096, 4096); w2s = wa(16384, 4096)
                return (w1s, w3s, w2s, w1s.ap.rearrange("p (k n) -> p k n", n=512), w3s.ap.rearrange("p (k n) -> p k n", n=512),
                        w2s.ap.rearrange("p (k n) -> p k n", n=1024))

            def moe_ld13(e):
                w1s, w3s, w2s, w1v, w3v, w2v = moe_w(e)
                ld("pool", w1v, w1_d[e].rearrange("(k p) n -> p k n", p=128), w1s.bufs[0], w1s.bufs)
                ld("pool", w3v, w3_d[e].rearrange("(k p) n -> p k n", p=128), w3s.bufs[0], w3s.bufs)

            def moe_ld2(e):
                w1s, w3s, w2s, w1v, w3v, w2v = moe_w(e)
                ld("pool", w2v, w2_d[e].rearrange("(k p) n -> p k n", p=128), w2s.bufs[0], w2s.bufs)

            moe_ld13(0)
            moe_ld2(0)
            for e in range(NE):
                w1s, w3s, w2s, w1v, w3v, w2v = moe_w(e)
                if e + 1 < NE:
                    moe_ld13(e + 1)
                for tb in range(NB):
                    at = AT[tb % 2]
                    at3 = at.ap.rearrange("p (f t) -> p f t", t=512)
                    for fc in range(4):
                        p1 = bank()
                        for kc in range(8):
                            mm(p1.ap, w1v[:, kc, fc * 128:(fc + 1) * 128], hT3[:, kc, tb * 512:(tb + 1) * 512], kc == 0, kc == 7,
                               w1s.bufs + hTb[tb * 4:tb * 4 + 4], p1.bufs)
                        p3 = bank()
                        for kc in range(8):
                            mm(p3.ap, w3v[:, kc, fc * 128:(fc + 1) * 128], hT3[:, kc, tb * 512:(tb + 1) * 512], kc == 0, kc == 7,
                               w3s.bufs + hTb[tb * 4:tb * 4 + 4], p3.bufs)
                        s1t = S1[fc % 2]
                        act(s1t.ap, p1.ap, AF.Silu, p1.bufs, s1t.bufs)
                        tt(at3[:, fc, :], s1t.ap, p3.ap, ALU.mult, s1t.bufs + p3.bufs, at.bufs)
                    for jj in range(4):
                        j = tb * 4 + jj
                        py = bank2()
                        for half in range(2):
                            for fc in range(4):
                                mm(py.ap[:, half * 512:(half + 1) * 512], at3[:, fc, jj * 128:(jj + 1) * 128],
                                   w2v[:, fc, half * 512:(half + 1) * 512], fc == 0, fc == 3, at.bufs + w2s.bufs, py.bufs)
                        acc = xacc(j)
                        if e == 0:
                            ts(acc.ap, py.ap, comb3[:, j, e:e + 1], None, ALU.mult, None, py.bufs + [bcomb], acc.bufs)
                        else:
                            stt(acc.ap, py.ap, comb3[:, j, e:e + 1], acc.ap, ALU.mult, ALU.add, py.bufs + [bcomb] + acc.bufs, acc.bufs)
                if e + 1 < NE:
                    moe_ld2(e + 1)

            if s == 0:
                prefetch_mixer_weights()
                phase1(1)
            make_gtbc(1, s)
            sq4 = small[:, 216:216 + NT]
            junk5 = ta(4096, 1024, BF16)
            for j in range(NT):
                act(junk5.ap, xacc(j).ap, AF.Square, xacc(j).bufs, junk5.bufs + [bsmall], accum_out=sq4[:, j:j + 1])
            act(sq4, sq4, AF.Sqrt, [bsmall], [bsmall], bias=EPS6, scale=1.0 / D)
            dve(lambda h: h.reciprocal(out=sq4, in_=sq4), [bsmall], [bsmall])
            X5 = [ta(5120, 1024), ta(6144, 1024)]
            O5 = [ta(7168, 1024), ta(8192, 1024)]
            bout = buf("out%d" % s)
            for j in range(NT):
                x5 = X5[j % 2]
                P.dma("sp", lambda h, x5=x5, j=j, s=s: h.dma_start(out=x5.ap, in_=x1s_d[s, j * 128:(j + 1) * 128, :]),
                      x5.bufs[0], r=[bx1], w=x5.bufs)
                o5 = O5[j % 2]
                stt(o5.ap, xacc(j).ap, sq4[:, j:j + 1], gtbc[:, D:2 * D], ALU.mult, ALU.mult, xacc(j).bufs + [bsmall, bgt[1]], o5.bufs)
                tt(o5.ap, o5.ap, x5.ap, ALU.add, o5.bufs + x5.bufs, o5.bufs)
                P.dma("sp", lambda h, o5=o5, j=j, s=s: h.dma_start(out=out_d[s, j * 128:(j + 1) * 128, :], in_=o5.ap),
                      bout, r=o5.bufs, w=[bout], is_out=True)

        if debug:
            P.dma("sp", lambda h: h.dma_start(out=dbg["hT"], in_=hT[:, :]), buf("dbg_hT"), r=hTb, w=[B["dbg_hT"]], is_out=True)
            if upto in (2, 3):
              P.dma("sp", lambda h: h.dma_start(out=dbg["yT"], in_=XA[:, NT * 1024:NT * 1024 + 8 * S]), buf("dbg_yT"), r=XAb, w=[B["dbg_yT"]], is_out=True)
            P.dma("sp", lambda h: h.dma_start(out=dbg["misc"][:, 0:96], in_=adaT[:, :]), buf("dbg_m"), r=[badaT], w=[B["dbg_m"]], is_out=True)
            P.dma("sp", lambda h: h.dma_start(out=dbg["misc"][:, 128:224], in_=scl[:, :]), B["dbg_m"], r=[bscl], w=[B["dbg_m"]], is_out=True)
            if upto >= 3:
              P.dma("sp", lambda h: h.dma_start(out=dbg["misc"][:, 256:256 + NT * 20], in_=lg[:, :]), B["dbg_m"], r=[blg], w=[B["dbg_m"]], is_out=True)
            if upto >= 4:
              P.dma("sp", lambda h: h.dma_start(out=dbg["misc"][:, 1024:1024 + NT * 16], in_=comb[:, :]), B["dbg_m"], r=[bcomb], w=[B["dbg_m"]], is_out=True)
            P.dma("sp", lambda h: h.dma_start(out=dbg["misc"][:, 2048:3072], in_=Rsb[:, :]), B["dbg_m"], r=[bconst], w=[B["dbg_m"]], is_out=True)

        P.lower(nc)
    return nc


def host_prep(inputs, S=2048):
    f = lambda a: np.ascontiguousarray(np.asarray(a, dtype=np.float32))
    w_in = f(inputs["w_in"][0])
    sl = [w_in[:, i * D:(i + 1) * D] for i in range(8)]
    u, v, q, ff_, i_, og, ga, gb = sl
    wfm = np.stack([np.concatenate([m[:, h * 128:(h + 1) * 128] for m in (u, ga, q, ff_, i_, og, gb)], axis=1)
                    for h in range(8)], axis=0)
    fm = lambda vec: f(vec).reshape(8, 128).T
    lb = f(inputs["lb_logits"])
    vecfm = np.zeros((128, 8, 8), np.float32)
    for k, vv in enumerate([inputs["g_pre_mix"][0], inputs["ln_v_g"][0], lb[0], lb[1], inputs["g_hgrn_norm"][0],
                            inputs["g_pre_ffn"][0], inputs["g_post_mix"][0], inputs["g_post_ffn"][0]]):
        vecfm[:, k, :] = fm(vv)
    cst = np.zeros((128, 1024), np.float32)
    cst[:, 0:128] = np.eye(128)
    cst[:, 128:256] = 1.0
    sidx = np.arange(128)[:, None]
    tidx = np.arange(128)[None, :]
    cst[:, 256:384] = ((sidx <= tidx) & (sidx // 64 == tidx // 64)).astype(np.float32)
    cst[:, 384:512] = (sidx <= tidx).astype(np.float32)
    cst[0, 512:640] = 1.0
    cst[1, 640:768] = 1.0
    shared = {
        "w_ada": f(inputs["w_ada"][0]),
        "b_ada2": np.ascontiguousarray(np.broadcast_to(f(inputs["b_ada"][0])[None, :], (2, 6 * D))),
        "vecfm": vecfm,
        "g_post_mix_bc": np.ascontiguousarray(np.broadcast_to(f(inputs["g_post_mix"][0])[None, :], (128, D))),
        "g_post_ffn_bc": np.ascontiguousarray(np.broadcast_to(f(inputs["g_post_ffn"][0])[None, :], (128, D))),
        "w_fm": np.ascontiguousarray(wfm),
        "w_v": np.ascontiguousarray(v),
        "w_spT": np.ascontiguousarray(np.transpose(f(inputs["w_spatial"][0]), (2, 0, 1))),
        "lnb2": np.ascontiguousarray(np.stack([f(inputs["ln_v_b"][0]), np.ones(D, np.float32)], axis=0)),
        "b_sp": f(inputs["b_spatial"][0]).reshape(1, 1024),
        "w_out": f(inputs["w_out"][0]),
        "w_r": np.ascontiguousarray(np.concatenate([f(inputs["w_router_group"][0]), f(inputs["w_router_expert"][0])], axis=1)),
        "b_r_bc": np.ascontiguousarray(np.broadcast_to(
            np.concatenate([f(inputs["b_router_group"][0]), f(inputs["b_router_expert"][0])])[None, :], (128, 20))),
        "w1": f(inputs["w1"][0]), "w3": f(inputs["w3"][0]), "w2": f(inputs["w2"][0]),
        "cst": cst,
    }
    x = f(inputs["x"])
    c = f(inputs["c"])
    ncore = x.shape[0] // 2
    maps = []
    for k in range(ncore):
        m = dict(shared)
        m["x"] = np.ascontiguousarray(x[2 * k:2 * k + 2, :S])
        cc = c[2 * k:2 * k + 2]
        m["cT"] = np.ascontiguousarray(cc.reshape(2, 8, 128).transpose(2, 1, 0))
        maps.append(m)
    return maps


def kernel(**inputs):
    maps = host_prep(inputs)
    nc = build()
    res = run_bass_kernel_spmd(nc, maps, core_ids=list(range(NCORES)))
    return np.concatenate([np.asarray(r["out"]) for r in res.results], axis=0).astype(np.float32)
```
